# Optimizing a Trainium2 kernel written in Bass

```python
import jax, jax.numpy as jnp
from jax import lax
import numpy as np

D_MODEL = 1024
BATCH = 4
SEQ = 8192
DEPTH = 2

N_MIXERS = 2
CONV_WIDTH = 3
ATTN_GROUPS = ((128, 1), (512, 4), (2048, 16))
N_GROUPS = len(ATTN_GROUPS)
HEADS_PER_GROUP = 5
HEAD_DIM = 64
ATTN_WIDTH = N_GROUPS * HEADS_PER_GROUP * HEAD_DIM
ROPE_THETA = 10000.0
D_FF = 2816
N_EXPERTS = 8
TOP_K = 2
D_FF_EXPERT = 2816
RMS_EPS = 1e-6
N_EVEN = (DEPTH + 1) // 2
N_ODD = DEPTH // 2

kernel_name = "hybrid_conv_dilated_attn_moe_adaln"


def rms_norm(x, g):
    xf = x.astype(jnp.float32)
    y = xf * lax.rsqrt(jnp.mean(xf * xf, axis=-1, keepdims=True) + RMS_EPS)
    return (y * g.astype(jnp.float32)).astype(x.dtype)


def ada_params(c, w, b):
    mod = jax.nn.silu(c) @ w + b
    shift, scale, gate = jnp.split(mod, 3, axis=-1)
    return shift[:, None, :], scale[:, None, :], gate[:, None, :]


def modulated_residual(x, c, g, w_mod, b_mod, fn):
    shift, scale, gate = ada_params(c, w_mod, b_mod)
    h = rms_norm(x, g) * (1 + scale) + shift
    return x + gate * fn(h)


def rope(x, positions):
    inv_freq = ROPE_THETA ** (-jnp.arange(0, HEAD_DIM, 2, dtype=jnp.float32) / HEAD_DIM)
    ang = positions.astype(jnp.float32)[..., None] * inv_freq
    cos, sin = jnp.cos(ang)[:, :, None, :], jnp.sin(ang)[:, :, None, :]
    xf = x.astype(jnp.float32)
    x1, x2 = jnp.split(xf, 2, axis=-1)
    return jnp.concatenate([x1 * cos - x2 * sin, x2 * cos + x1 * sin], axis=-1).astype(x.dtype)


def short_conv_mixer(h, w_in, conv_w, w_out):
    b_gate, c_gate, u = jnp.split(h @ w_in, 3, axis=-1)
    v = c_gate * u
    conv = lax.conv_general_dilated(
        v, conv_w.astype(v.dtype)[:, None, :], window_strides=(1,),
        padding=[(CONV_WIDTH - 1, 0)], dimension_numbers=('NWC', 'WIO', 'NWC'),
        feature_group_count=v.shape[-1])
    return (b_gate * conv) @ w_out


def dilated_group_attention(q, k, v, dilation, steps):
    b, s, nh, dh = q.shape
    chunk = dilation * steps
    sp = -(-s // chunk) * chunk
    nb = sp // chunk
    pad = [(0, 0), (0, sp - s), (0, 0), (0, 0)]

    def to_blocks(t):
        return jnp.pad(t, pad).reshape(b, nb, steps, dilation, nh, dh)

    def with_prev(t):
        prev = jnp.pad(t[:, :-1], [(0, 0), (1, 0), (0, 0), (0, 0), (0, 0), (0, 0)])
        return jnp.concatenate([prev, t], axis=2)

    qb = to_blocks(q)
    kk = with_prev(to_blocks(k))
    vv = with_prev(to_blocks(v))
    scores = jnp.einsum('bnqrhd,bnkrhd->bnrhqk', qb, kk,
                        preferred_element_type=jnp.float32) * (dh ** -0.5)
    a = jnp.arange(steps)[:, None]
    j = jnp.arange(2 * steps)[None, :]
    band = (j >= a) & (j <= a + steps)
    not_first = jnp.arange(nb)[:, None, None] > 0
    valid = band[None] & (not_first | (j >= steps)[None])
    scores = jnp.where(valid[None, :, None, None], scores, -jnp.inf)
    lse = jax.nn.logsumexp(scores, axis=-1)
    p = jnp.exp(scores - lse[..., None])
    o = jnp.einsum('bnrhqk,bnkrhd->bnqrhd', p.astype(v.dtype), vv)
    o = o.reshape(b, sp, nh, dh)[:, :s]
    lse = lse.transpose(0, 1, 4, 2, 3).reshape(b, sp, nh)[:, :s]
    return o, lse


def dilated_attention_mixer(h, positions, w_qkv, w_o):
    b, s, _ = h.shape
    nh = N_GROUPS * HEADS_PER_GROUP
    qkv = (h @ w_qkv).reshape(b, s, 3, nh, HEAD_DIM)
    q = rope(qkv[:, :, 0], positions).reshape(b, s, N_GROUPS, HEADS_PER_GROUP, HEAD_DIM)
    k = rope(qkv[:, :, 1], positions).reshape(b, s, N_GROUPS, HEADS_PER_GROUP, HEAD_DIM)
    v = qkv[:, :, 2].reshape(b, s, N_GROUPS, HEADS_PER_GROUP, HEAD_DIM)
    outs, lses = [], []
    for g, (window, dil) in enumerate(ATTN_GROUPS):
        o, l = dilated_group_attention(q[:, :, g], k[:, :, g], v[:, :, g], dil, window // dil)
        outs.append(o)
        lses.append(l)
    alpha = jax.nn.softmax(jnp.stack(lses, axis=2), axis=2)
    o = jnp.stack(outs, axis=2) * alpha[..., None].astype(h.dtype)
    return o.reshape(b, s, ATTN_WIDTH) @ w_o


def swiglu(h, w_gate, w_up, w_down):
    return (jax.nn.silu(h @ w_gate) * (h @ w_up)) @ w_down


def moe_swiglu(h, w_router, w_gate, w_up, w_down):
    logits = jnp.einsum('bsd,de->bse', h, w_router, preferred_element_type=jnp.float32)
    top_val, top_idx = lax.top_k(logits, TOP_K)
    top_w = jax.nn.softmax(top_val, axis=-1)
    gates = jnp.sum(jax.nn.one_hot(top_idx, N_EXPERTS, dtype=jnp.float32) * top_w[..., None],
                    axis=-2).astype(h.dtype)
    out = jnp.zeros_like(h)
    for e in range(N_EXPERTS):
        out = out + gates[..., e:e + 1] * swiglu(h, w_gate[e], w_up[e], w_down[e])
    return out


def setup_inputs(seed: int = 0) -> dict:
    key = jax.random.key(seed)
    ks = jax.random.split(key, 20)
    D, F, FE, E = D_MODEL, D_FF, D_FF_EXPERT, N_EXPERTS

    def w(k, shape, fan_in, scale=1.0):
        return jax.random.normal(k, shape, jnp.float32) * (scale * fan_in ** -0.5)

    offsets = jax.random.randint(ks[2], (BATCH, 1), 0, 1024, dtype=jnp.int32)
    return {
        "x": jax.random.normal(ks[0], (BATCH, SEQ, D), jnp.float32),
        "c": jax.random.normal(ks[1], (BATCH, D), jnp.float32),
        "positions": offsets + jnp.arange(SEQ, dtype=jnp.int32)[None, :],
        "mod_w": w(ks[3], (DEPTH, 2, D, 3 * D), D, 0.5),
        "mod_b": 0.02 * jax.random.normal(ks[4], (DEPTH, 2, 3 * D), jnp.float32),
        "norm_g": 1.0 + 0.05 * jax.random.normal(ks[5], (DEPTH, 2, D), jnp.float32),
        "conv_w_in": w(ks[6], (N_EVEN, D, 3 * D), D),
        "conv_w": w(ks[7], (N_EVEN, CONV_WIDTH, D), CONV_WIDTH),
        "conv_w_out": w(ks[8], (N_EVEN, D, D), D),
        "ffn_w_gate": w(ks[9], (N_EVEN, D, F), D),
        "ffn_w_up": w(ks[10], (N_EVEN, D, F), D),
        "ffn_w_down": w(ks[11], (N_EVEN, F, D), F),
        "attn_w_qkv": w(ks[12], (N_ODD, D, 3 * ATTN_WIDTH), D),
        "attn_w_o": w(ks[13], (N_ODD, ATTN_WIDTH, D), ATTN_WIDTH),
        "router_w": w(ks[14], (N_ODD, D, E), D),
        "moe_w_gate": w(ks[15], (N_ODD, E, D, FE), D),
        "moe_w_up": w(ks[16], (N_ODD, E, D, FE), D),
        "moe_w_down": w(ks[17], (N_ODD, E, FE, D), FE),
        "final_g": 1.0 + 0.05 * jax.random.normal(ks[18], (D,), jnp.float32),
    }


def reference(x, c, positions, mod_w, mod_b, norm_g, conv_w_in, conv_w, conv_w_out,
              ffn_w_gate, ffn_w_up, ffn_w_down, attn_w_qkv, attn_w_o, router_w,
              moe_w_gate, moe_w_up, moe_w_down, final_g):
    c = c.astype(x.dtype)
    for i in range(DEPTH):
        j = i // N_MIXERS
        if i % N_MIXERS == 0:
            mixer = lambda h, j=j: short_conv_mixer(h, conv_w_in[j], conv_w[j], conv_w_out[j])
            channel = lambda h, j=j: swiglu(h, ffn_w_gate[j], ffn_w_up[j], ffn_w_down[j])
        else:
            mixer = lambda h, j=j: dilated_attention_mixer(h, positions, attn_w_qkv[j], attn_w_o[j])
            channel = lambda h, j=j: moe_swiglu(h, router_w[j], moe_w_gate[j], moe_w_up[j],
                                                moe_w_down[j])
        x = modulated_residual(x, c, norm_g[i, 0], mod_w[i, 0], mod_b[i, 0], mixer)
        x = modulated_residual(x, c, norm_g[i, 1], mod_w[i, 1], mod_b[i, 1], channel)
    return rms_norm(x, final_g)
```

```python
import types
import numpy as np
from contextlib import ExitStack
import concourse.bass as bass
import concourse.mybir as mybir
from concourse.bass_utils import run_bass_kernel_spmd

F32 = mybir.dt.float32
BF16 = mybir.dt.bfloat16
I32 = mybir.dt.int32
AF = mybir.ActivationFunctionType
ALU = mybir.AluOpType

D = 1024
KC = 8
FF = 2816
FC = 22
NE = 8
TB = 2048
TL = 512
NS = 6656
NKV = 6144
NOWN = 4096
EPS = 1e-6
TWO_PI = float(2 * np.pi)


class Buf:
    __slots__ = ("name", "lw", "rde", "rdd", "dsem", "dcnt", "ps")

    def __init__(self, name=""):
        self.name = name
        self.lw = None
        self.rde = {}
        self.rdd = []
        self.dsem = None
        self.dcnt = 0
        self.ps = False


def _freeze(fn):
    if fn is None or fn.__closure__ is None:
        return fn
    cells = []
    for c in fn.__closure__:
        try:
            cells.append(types.CellType(c.cell_contents))
        except ValueError:
            cells.append(c)
    return types.FunctionType(fn.__code__, fn.__globals__, fn.__name__, fn.__defaults__, tuple(cells))


class Sched:
    ENGS = ("pe", "act", "dve", "pool", "sp")

    def __init__(self, nc):
        self.nc = nc
        self.ops = {e: [] for e in self.ENGS}
        self.seen = {e: {} for e in self.ENGS}
        self.ref = {e: set() for e in self.ENGS}
        self.ndsem = 0
        self.dma_tokens = []
        self.lastc = {e: 0 for e in self.ENGS}

    def _need(self, eng, tok, waits):
        if tok is None:
            return
        if tok[0] == 'e':
            _, pe_, idx = tok
            if pe_ == eng and eng == "pe":
                return
            if self.seen[eng].get(pe_, 0) >= idx:
                return
            self.seen[eng][pe_] = idx
            self.ref[pe_].add(idx)
            waits.append(tok)
        else:
            _, s, v = tok
            key = ('d', s)
            if self.seen[eng].get(key, 0) >= v:
                return
            self.seen[eng][key] = v
            waits.append(tok)

    def _hazards(self, eng, reads, writes):
        waits = []
        for b in reads:
            self._need(eng, b.lw, waits)
            if b.ps:
                for e2, i2 in b.rde.items():
                    if e2 != eng:
                        self._need(eng, ('e', e2, i2), waits)
        for b in writes:
            self._need(eng, b.lw, waits)
            for e2, i2 in b.rde.items():
                self._need(eng, ('e', e2, i2), waits)
            for t in b.rdd:
                self._need(eng, t, waits)
        return waits

    def op(self, eng, fn, reads=(), writes=()):
        waits = self._hazards(eng, reads, writes)
        idx = len(self.ops[eng]) + 1
        tok = ('e', eng, idx)
        self.ops[eng].append([waits, _freeze(fn), False, None])
        self.lastc[eng] = idx
        for b in reads:
            b.rde[eng] = idx
        for b in writes:
            b.lw = tok
            b.rde = {}
            b.rdd = []
        return tok

    def dma(self, eng, fn, reads=(), writes=()):
        waits = self._hazards(eng, reads, writes)
        owner = writes[0] if writes else reads[0]
        if owner.dsem is None:
            owner.dsem = self.ndsem
            self.ndsem += 1
        owner.dcnt += 16
        tok = ('d', owner.dsem, owner.dcnt)
        self.ops[eng].append([waits, _freeze(fn), True, owner.dsem])
        for b in reads:
            b.rdd.append(tok)
        for b in writes:
            b.lw = tok
            b.rde = {}
            b.rdd = []
        if eng == "sp":
            self.dma_tokens.append(tok)
        return tok

    def barrier(self, engs=("pe", "act", "dve", "sp")):
        last = {e: self.lastc[e] for e in ("pe", "act", "dve", "pool")}
        toks = self.dma_tokens
        self.dma_tokens = []
        for e in engs:
            waits = []
            for e2, i2 in last.items():
                if e2 != e and i2 > 0:
                    self._need(e, ('e', e2, i2), waits)
            for t in toks:
                self._need(e, t, waits)
            if waits:
                self.ops[e].append([waits, None, False, None])

    def final_wait(self, eng, bufs):
        waits = []
        for b in bufs:
            self._need(eng, b.lw, waits)
            for t in b.rdd:
                self._need(eng, t, waits)
        self.ops[eng].append([waits, None, False, None])

    def emit(self, stack):
        nc = self.nc
        print("ndsem", self.ndsem)
        esem = {e: stack.enter_context(nc.semaphore("es_" + e)) for e in self.ENGS}
        dsem = [stack.enter_context(nc.semaphore("ds%d" % i)) for i in range(self.ndsem)]
        cum = {}
        for e in self.ENGS:
            c = 0
            arr = [0]
            for i in range(1, len(self.ops[e]) + 1):
                if i in self.ref[e]:
                    c += 1
                arr.append(c)
            cum[e] = arr
        block = stack.enter_context(nc.Block())
        handles = {"pe": block.tensor, "act": block.scalar, "dve": block.vector,
                   "pool": block.gpsimd, "sp": block.sync}

        def mk(e):
            def body(h):
                for i, (waits, fn, is_dma, ds) in enumerate(self.ops[e], start=1):
                    for t in waits:
                        if t[0] == 'e':
                            h.wait_ge(esem[t[1]], cum[t[1]][t[2]])
                        else:
                            h.wait_ge(dsem[t[1]], t[2])
                    if fn is None:
                        continue
                    ins = fn(h)
                    if is_dma:
                        ins.then_inc(dsem[ds], 16)
                    elif i in self.ref[e]:
                        ins.then_inc(esem[e], 1)
            return body

        for e in self.ENGS:
            handles[e](mk(e))
        return {e: (len(self.ops[e]), cum[e][-1]) for e in self.ENGS}


class Arena:
    def __init__(self, nc, start, end):
        self.nc, self.ptr, self.end, self.n = nc, start, end, 0

    def alloc(self, shape, dt):
        es = 4 if dt in (F32, I32) else 2
        nbytes = int(np.prod(shape[1:])) * es
        addr = (self.ptr + 31) // 32 * 32
        assert addr + nbytes <= self.end, ("SBUF overflow", addr, nbytes, self.end)
        self.ptr = addr + nbytes
        self.n += 1
        return self.nc.alloc_sbuf_tensor_at("a%d" % self.n, list(shape), dt, offset=addr).ap()

    def mark(self):
        return self.ptr

    def release(self, m):
        self.ptr = m


def build_program(stop_after=None):
    nc = bass.Bass("TRN2", target_bir_lowering=False)

    used = []

    class Lazy:
        def __init__(self, name, shape, dt):
            self.a = (name, list(shape), dt)
            self.v = None

        def get(self):
            if self.v is None:
                self.v = nc.dram_tensor(self.a[0], self.a[1], self.a[2], kind="ExternalInput").ap()
                used.append(self.a[0])
            return self.v

        def __getitem__(self, key):
            return self.get()[key]

        def rearrange(self, *a, **k):
            return self.get().rearrange(*a, **k)

    def din(name, shape, dt=F32):
        return Lazy(name, shape, dt)

    xs = din("xs", [NS, D])
    poss = din("poss", [1, NS], I32)
    cols = din("cols", [128, 176])
    cmat = din("cmat", [128, 128 + 256 + 64])
    sel = din("sel", [8, NE * 128])
    modw = din("modw", [4 * D, 3 * D])
    w_cin = din("w_cin", [D, 3 * D])
    w_cout = din("w_cout", [D, D])
    w_fg = din("w_fg", [D, FF])
    w_fu = din("w_fu", [D, FF])
    w_fd = din("w_fd", [FF, D])
    w_q = din("w_q", [D, D])
    w_qs = din("w_qs", [D, D])
    w_k = din("w_k", [D, D])
    w_ks = din("w_ks", [D, D])
    w_v = din("w_v", [D, D])
    w_o = din("w_o", [D, D])
    w_r = din("w_r", [D, NE])
    w_mg = din("w_mg", [NE * D, FF])
    w_mu = din("w_mu", [NE * D, FF])
    w_md = din("w_md", [NE * FF, D])
    out = nc.dram_tensor("out", [NOWN, D], F32, kind="ExternalOutput").ap()
    PAD0 = nc.dram_tensor("PAD0", [128, 1024], F32, kind="Internal").ap()
    QKT = nc.dram_tensor("QKT", [3 * D, NKV], BF16, kind="Internal").ap()
    KT = QKT[0:D, :]
    QT = QKT[D:2 * D, 0:NOWN]
    VV = nc.dram_tensor("VV", [NKV, D], BF16, kind="Internal").ap()
    AT = QKT[2 * D:3 * D, 0:NOWN]
    _q1, _k1, _a1, _v1 = Buf(), Buf(), Buf(), Buf()
    _q = [_q1, _q1]
    _k = [_k1, _k1, _k1]
    _a = [_a1, _a1]
    bQT = [_q for _ in range(KC)]
    bKT = [_k for _ in range(KC)]
    bVV = [_v1, _v1, _v1]
    bAT = [_a for _ in range(16)]
    _gb = {}

    def gbuf(name):
        if name not in _gb:
            _gb[name] = Buf(name)
        return _gb[name]

    S = Sched(nc)
    st = ExitStack()
    A = Arena(nc, 16512, 229376 - 64)

    xT = A.alloc([128, KC, TB], F32)
    bxT = [[Buf() for _ in range(4)] for _ in range(KC)]
    hT_addr = (A.ptr + 31) // 32 * 32
    hT = A.alloc([128, KC, TB], BF16)
    bhT = [[Buf() for _ in range(4)] for _ in range(KC)]
    COLS = A.alloc([128, 176], F32); bCOLS = Buf()
    CM = A.alloc([128, 448], F32); bCM = Buf()
    IDN = CM[:, 0:128]
    ONES = A.alloc([128, 128], F32); bONES = Buf()
    ONESB = A.alloc([128, 64], BF16)
    MASK = A.alloc([128, 256], BF16); bMASK = Buf()
    MASKH = A.alloc([128, 256], BF16)
    SEL = A.alloc([8, NE * 128], F32); bSEL = Buf()
    MODC = A.alloc([128, 96], F32); bMODC = Buf()
    GS = A.alloc([128, 32], F32)
    WR = A.alloc([128, KC, NE], BF16); bWR = Buf()
    VCAR = A.alloc([128, KC, 2], F32); bVCAR = [Buf() for _ in range(KC)]
    C_NG, C_FG, C_CW, C_IF, C_SG, C_FL, C_C, C_MB = 0, 32, 40, 64, 65, 66, 67, 75
    EPSC = A.alloc([128, 1], F32)
    NSLOT = 2
    RING = [A.alloc([128, 12288], BF16) for _ in range(NSLOT)]
    bRING = [Buf() for _ in range(NSLOT)]
    ring_ctr = [0]

    def ring_next():
        i = ring_ctr[0] % NSLOT
        ring_ctr[0] += 1
        return RING[i], bRING[i]

    PS = [nc.alloc_psum_tensor("ps%d" % i, [128, 512], F32).ap() for i in range(8)]
    bPS = [Buf() for _ in range(8)]
    for b_ in bPS:
        b_.ps = True
    ps_ctr = [0]

    def ps_next():
        i = ps_ctr[0] % 8
        ps_ctr[0] += 1
        return PS[i], bPS[i]

    S.dma("sp", lambda h: h.dma_start(out=COLS, in_=cols[:, :]), writes=[bCOLS])
    S.dma("sp", lambda h: h.dma_start(out=CM, in_=cmat[:, :]), writes=[bCM])
    S.dma("sp", lambda h: h.dma_start(out=SEL, in_=sel[:, :]), writes=[bSEL])
    S.dma("sp", lambda h: h.dma_start(out=PAD0[:, 0:176], in_=COLS), reads=[bCOLS], writes=[Buf("pad0")])
    S.dma("pool", lambda h: h.dma_start(out=WR, in_=w_r.rearrange("(k p) e -> p k e", p=128)), writes=[bWR])
    S.op("dve", lambda h: h.memset(ONES, 1.0), writes=[bONES])
    S.op("dve", lambda h: h.memset(ONESB, 1.0), writes=[bONES])
    S.op("dve", lambda h: h.memset(EPSC, EPS), writes=[bONES])
    S.op("dve", lambda h: h.tensor_copy(MASK, CM[:, 128:384]), reads=[bCM], writes=[bMASK])
    S.op("dve", lambda h: h.tensor_copy(MASKH[:, 128:256], CM[:, 256:384]), reads=[bCM], writes=[bMASK])
    S.op("dve", lambda h: h.tensor_scalar_mul(MASKH[:, 0:128], CM[:, 128:256], COLS[:, C_FL:C_FL + 1]),
         reads=[bCM, bCOLS], writes=[bMASK])
    for k in range(KC):
        S.op("dve", lambda h, k=k: h.memset(VCAR[:, k, :], 0.0), writes=[bVCAR[k]])

    m0 = A.mark()
    SC = A.alloc([128, KC], F32); bSC = Buf()
    S.op("act", lambda h: h.activation(out=SC, in_=COLS[:, C_C:C_C + 8], func=AF.Silu), reads=[bCOLS], writes=[bSC])
    MW = [A.alloc([128, KC, 512], F32) for _ in range(2)]
    bMW = [Buf(), Buf()]
    psm, bpsm = ps_next()
    gi = 0
    for s_ in range(4):
        for cg in range(6):
            mw, bmw = MW[gi % 2], bMW[gi % 2]
            gi += 1
            src = modw[s_ * D:(s_ + 1) * D, cg * 512:(cg + 1) * 512].rearrange("(k p) n -> p k n", p=128)
            S.dma("sp", lambda h, mw=mw, src=src: h.dma_start(out=mw, in_=src), writes=[bmw])
            for jj in range(4):
                col = s_ * 24 + cg * 4 + jj
                for k in range(KC):
                    S.op("pe", lambda h, mw=mw, jj=jj, k=k, col=col: h.matmul(
                        psm[:, col:col + 1], mw[:, k, jj * 128:(jj + 1) * 128], SC[:, k:k + 1],
                        start=(k == 0), stop=(k == KC - 1)), reads=[bmw, bSC], writes=[bpsm])
    S.op("dve", lambda h: h.tensor_tensor(MODC, psm[:, 0:96], COLS[:, C_MB:C_MB + 96], op=ALU.add),
         reads=[bpsm, bCOLS], writes=[bMODC])
    for s_ in range(4):
        S.op("dve", lambda h, s_=s_: h.tensor_scalar_add(GS[:, s_ * 8:(s_ + 1) * 8], MODC[:, s_ * 24 + 8:s_ * 24 + 16], 1.0),
             reads=[bMODC], writes=[bMODC])
        S.op("dve", lambda h, s_=s_: h.tensor_mul(GS[:, s_ * 8:(s_ + 1) * 8], GS[:, s_ * 8:(s_ + 1) * 8],
                                                  COLS[:, C_NG + s_ * 8:C_NG + (s_ + 1) * 8]),
             reads=[bMODC, bCOLS], writes=[bMODC])
    S.barrier()
    A.release(m0)

    def shiftc(s_, k):
        return MODC[:, s_ * 24 + k:s_ * 24 + k + 1]

    def gatec(s_, k):
        return MODC[:, s_ * 24 + 16 + k:s_ * 24 + 16 + k + 1]

    def gsc(s_, k):
        return GS[:, s_ * 8 + k:s_ * 8 + k + 1]

    def load_x(s0, ntiles):
        m = A.mark()
        XL = [A.alloc([128, 4, D], F32) for _ in range(2)]
        bXL = [gbuf("xl0"), gbuf("xl1")]
        for t in range(ntiles):
            xl, bxl = XL[t % 2], bXL[t % 2]
            src = xs[s0 + t * TL:s0 + (t + 1) * TL, :].rearrange("(a p) f -> p a f", p=128)
            S.dma("sp", lambda h, xl=xl, src=src: h.dma_start(out=xl, in_=src), writes=[bxl])
            for k in range(KC):
                ps, bps = ps_next()
                for a in range(4):
                    S.op("pe", lambda h, ps=ps, xl=xl, a=a, k=k: h.transpose(
                        ps[:, a * 128:(a + 1) * 128], xl[:, a, k * 128:(k + 1) * 128], IDN),
                        reads=[bxl, bCM], writes=[bps])
                dst = xT[:, k, t * TL:(t + 1) * TL]
                if k % 2 == 0:
                    S.op("act", lambda h, ps=ps, dst=dst: h.copy(dst, ps), reads=[bps], writes=[bxT[k][t]])
                else:
                    S.op("dve", lambda h, ps=ps, dst=dst: h.tensor_copy(dst, ps), reads=[bps], writes=[bxT[k][t]])
        S.barrier()
        A.release(m)

    def norm_stats(t, SQ, bSQ, RS, bRS):
        ps, bps = ps_next()
        for k in range(KC):
            sq, bsq = SQ[k % 2], bSQ[k % 2]
            S.op("act", lambda h, sq=sq, k=k: h.activation(out=sq, in_=xT[:, k, t * TL:(t + 1) * TL], func=AF.Square),
                 reads=[bxT[k][t]], writes=[bsq])
            S.op("pe", lambda h, ps=ps, sq=sq, k=k: h.matmul(ps, ONES, sq, start=(k == 0), stop=(k == KC - 1)),
                 reads=[bsq, bONES], writes=[bps])
        S.op("act", lambda h, ps=ps: h.activation(out=RS, in_=ps, func=AF.Sqrt, bias=EPSC[:, 0:1], scale=1.0 / D),
             reads=[bps, bONES], writes=[bRS])
        S.op("dve", lambda h: h.reciprocal(RS, RS), reads=[bRS], writes=[bRS])

    def norm_mod(s_, ntiles):
        m = A.mark()
        SQ = [A.alloc([128, TL], F32) for _ in range(2)]
        bSQ = [Buf(), Buf()]
        RS = [A.alloc([128, TL], F32) for _ in range(2)]
        bRS = [Buf(), Buf()]
        TM = [A.alloc([128, TL], F32) for _ in range(2)]
        bTM = [Buf(), Buf()]
        for t in range(ntiles):
            rs, brs = RS[t % 2], bRS[t % 2]
            norm_stats(t, SQ, bSQ, rs, brs)
            for k in range(KC):
                tm, btm = TM[k % 2], bTM[k % 2]
                S.op("dve", lambda h, tm=tm, k=k, rs=rs: h.scalar_tensor_tensor(
                    out=tm, in0=xT[:, k, t * TL:(t + 1) * TL], scalar=gsc(s_, k), in1=rs, op0=ALU.mult, op1=ALU.mult),
                    reads=[bxT[k][t], brs, bMODC], writes=[btm])
                S.op("act", lambda h, tm=tm, k=k: h.activation(
                    out=hT[:, k, t * TL:(t + 1) * TL], in_=tm, func=AF.Identity, bias=shiftc(s_, k), scale=1.0),
                    reads=[btm, bMODC], writes=[bhT[k][t]])
        S.barrier()
        A.release(m)

    def wload(dst, src, bslot):
        S.dma("pool", lambda h: h.dma_start(out=dst, in_=src), writes=[bslot])

    def kview(w, c0, c1):
        return w[:, c0:c1].rearrange("(k p) n -> p k n", p=128)

    def resid_add(ps, bps, s_, dch, t):
        dst = xT[:, dch, t * TL:(t + 1) * TL]
        S.op("dve", lambda h: h.scalar_tensor_tensor(out=dst, in0=ps, scalar=gatec(s_, dch), in1=dst,
                                                     op0=ALU.mult, op1=ALU.add),
             reads=[bps, bMODC], writes=[bxT[dch][t]])

    def conv_phase(ntiles, v_only, first_flag):
        s_ = 0
        m = A.mark()
        VB = [A.alloc([128, TL + 2], F32) for _ in range(2)]
        bVB = [Buf(), Buf()]
        CG = [A.alloc([128, TL], F32) for _ in range(2)]
        bCG = [Buf(), Buf()]
        AC = [A.alloc([128, TL], F32) for _ in range(2)]
        bAC = [Buf(), Buf()]
        MT = [A.alloc([128, 2, TL], BF16) for _ in range(2)]
        bMT = [Buf(), Buf()]
        if first_flag:
            for k in range(KC):
                S.op("dve", lambda h, k=k: h.tensor_scalar_mul(VCAR[:, k, :], VCAR[:, k, :], COLS[:, C_FL:C_FL + 1]),
                     reads=[bCOLS], writes=[bVCAR[k]])
        cnt = 0
        for jg in range(4):
            slot, bslot = ring_next()
            WI = slot[:, 0:KC * 768].rearrange("p (k n) -> p k n", k=KC)
            WO = slot[:, 6144:6144 + 2 * D].rearrange("p (j n) -> p j n", j=2)
            for jl in range(2):
                j = jg * 2 + jl
                for part in range(3):
                    wload(WI[:, :, (jl * 3 + part) * 128:(jl * 3 + part + 1) * 128],
                          kview(w_cin, part * D + j * 128, part * D + (j + 1) * 128), bslot)
            if not v_only:
                wload(WO, w_cout[jg * 256:(jg + 1) * 256, :].rearrange("(j p) n -> p j n", p=128), bslot)
            for t in range(ntiles):
                mt, bmt = MT[(jg * ntiles + t) % 2], bMT[(jg * ntiles + t) % 2]
                for jl in range(2):
                    j = jg * 2 + jl
                    vb, bvb = VB[cnt % 2], bVB[cnt % 2]
                    cgb, bcgb = CG[cnt % 2], bCG[cnt % 2]
                    ac, bac = AC[cnt % 2], bAC[cnt % 2]
                    cnt += 1
                    pss = []
                    for part in range(3):
                        if v_only and part == 0:
                            pss.append((None, None))
                            continue
                        ps, bps = ps_next()
                        for k in range(KC):
                            S.op("pe", lambda h, ps=ps, k=k, part=part, jl=jl: h.matmul(
                                ps, WI[:, k, (jl * 3 + part) * 128:(jl * 3 + part + 1) * 128], hT[:, k, t * TL:(t + 1) * TL],
                                start=(k == 0), stop=(k == KC - 1)), reads=[bslot, bhT[k][t]], writes=[bps])
                        pss.append((ps, bps))
                    (psb, bpsb), (psc, bpsc), (psu, bpsu) = pss
                    S.op("act", lambda h, cgb=cgb, psc=psc: h.copy(cgb, psc), reads=[bpsc], writes=[bcgb])
                    S.op("dve", lambda h, vb=vb, j=j: h.tensor_copy(vb[:, 0:2], VCAR[:, j, :]), reads=[bVCAR[j]], writes=[bvb])
                    S.op("dve", lambda h, vb=vb, cgb=cgb, psu=psu: h.tensor_tensor(vb[:, 2:TL + 2], psu, cgb, op=ALU.mult),
                         reads=[bpsu, bcgb], writes=[bvb])
                    S.op("dve", lambda h, vb=vb, j=j: h.tensor_copy(VCAR[:, j, :], vb[:, TL:TL + 2]), reads=[bvb], writes=[bVCAR[j]])
                    if v_only:
                        continue
                    cw = lambda tap, j=j: COLS[:, C_CW + tap * 8 + j:C_CW + tap * 8 + j + 1]
                    S.op("dve", lambda h, ac=ac, vb=vb, cw=cw: h.tensor_scalar_mul(ac, vb[:, 2:TL + 2], cw(2)),
                         reads=[bvb, bCOLS], writes=[bac])
                    S.op("dve", lambda h, ac=ac, vb=vb, cw=cw: h.scalar_tensor_tensor(
                        out=ac, in0=vb[:, 1:TL + 1], scalar=cw(1), in1=ac, op0=ALU.mult, op1=ALU.add),
                        reads=[bvb, bCOLS], writes=[bac])
                    S.op("dve", lambda h, ac=ac, vb=vb, cw=cw: h.scalar_tensor_tensor(
                        out=ac, in0=vb[:, 0:TL], scalar=cw(0), in1=ac, op0=ALU.mult, op1=ALU.add),
                        reads=[bvb, bCOLS], writes=[bac])
                    S.op("dve", lambda h, mt=mt, jl=jl, psb=psb, ac=ac: h.tensor_tensor(mt[:, jl, :], psb, ac, op=ALU.mult),
                         reads=[bpsb, bac], writes=[bmt])
                if v_only:
                    continue
                for dch in range(KC):
                    ps, bps = ps_next()
                    for jl in range(2):
                        S.op("pe", lambda h, ps=ps, jl=jl, dch=dch, mt=mt: h.matmul(
                            ps, WO[:, jl, dch * 128:(dch + 1) * 128], mt[:, jl, :], start=(jl == 0), stop=(jl == 1)),
                            reads=[bslot, bmt], writes=[bps])
                    resid_add(ps, bps, s_, dch, t)
        S.barrier()
        A.release(m)

    def swiglu(s_, wg, wu, wd, row0g, row0d, G, bG, AP_, bAP_, SGt, bSGt, UGt, bUGt, pend):
        groups = [(0, 4), (4, 4), (8, 4), (12, 4), (16, 4), (20, 2)]
        for (f0, fg) in groups:
            slot, bslot = ring_next()
            WG = slot[:, 0:KC * fg * 128].rearrange("p (k n) -> p k n", k=KC)
            WU = slot[:, 4096:4096 + KC * fg * 128].rearrange("p (k n) -> p k n", k=KC)
            WD = slot[:, 8192:8192 + fg * D].rearrange("p (f n) -> p f n", f=fg)
            wload(WG, wg[row0g:row0g + D, f0 * 128:(f0 + fg) * 128].rearrange("(k p) n -> p k n", p=128), bslot)
            wload(WU, wu[row0g:row0g + D, f0 * 128:(f0 + fg) * 128].rearrange("(k p) n -> p k n", p=128), bslot)
            wload(WD, wd[row0d + f0 * 128:row0d + (f0 + fg) * 128, :].rearrange("(f p) n -> p f n", p=128), bslot)
            for t in range(4):
                ap_, bap_ = AP_[pend["n"] % 2], bAP_[pend["n"] % 2]
                pend["n"] += 1
                for fi in range(fg):
                    psg, bpsg = ps_next()
                    for k in range(KC):
                        S.op("pe", lambda h, psg=psg, k=k, fi=fi, WG=WG: h.matmul(
                            psg, WG[:, k, fi * 128:(fi + 1) * 128], hT[:, k, t * TL:(t + 1) * TL],
                            start=(k == 0), stop=(k == KC - 1)), reads=[bslot, bhT[k][t]], writes=[bpsg])
                    psu, bpsu = ps_next()
                    for k in range(KC):
                        S.op("pe", lambda h, psu=psu, k=k, fi=fi, WU=WU: h.matmul(
                            psu, WU[:, k, fi * 128:(fi + 1) * 128], hT[:, k, t * TL:(t + 1) * TL],
                            start=(k == 0), stop=(k == KC - 1)), reads=[bslot, bhT[k][t]], writes=[bpsu])
                    i2 = pend["m"] % 2
                    pend["m"] += 1
                    sg, bsg = SGt[i2], bSGt[i2]
                    S.op("act", lambda h, sg=sg, psg=psg: h.activation(out=sg, in_=psg, func=AF.Silu),
                         reads=[bpsg], writes=[bsg])
                    if G is not None:
                        ug, bug = UGt[i2], bUGt[i2]
                        S.op("dve", lambda h, ug=ug, psu=psu: h.tensor_tensor(ug, psu, G[:, t * TL:(t + 1) * TL], op=ALU.mult),
                             reads=[bpsu, bG], writes=[bug])
                        S.op("dve", lambda h, ap_=ap_, fi=fi, sg=sg, ug=ug: h.tensor_tensor(ap_[:, fi, :], sg, ug, op=ALU.mult),
                             reads=[bsg, bug], writes=[bap_])
                    else:
                        S.op("dve", lambda h, ap_=ap_, fi=fi, sg=sg, psu=psu: h.tensor_tensor(ap_[:, fi, :], psu, sg, op=ALU.mult),
                             reads=[bpsu, bsg], writes=[bap_])
                if pend["fn"] is not None:
                    pend["fn"]()

                def ydown(t=t, fg=fg, WD=WD, ap_=ap_, bap_=bap_, bslot=bslot):
                    for dch in range(KC):
                        ps, bps = ps_next()
                        for fi in range(fg):
                            S.op("pe", lambda h, ps=ps, fi=fi, dch=dch: h.matmul(
                                ps, WD[:, fi, dch * 128:(dch + 1) * 128], ap_[:, fi, :], start=(fi == 0), stop=(fi == fg - 1)),
                                reads=[bslot, bap_], writes=[bps])
                        resid_add(ps, bps, s_, dch, t)
                pend["fn"] = ydown

    def ffn_phase():
        s_ = 1
        norm_mod(s_, 4)
        m = A.mark()
        AP_ = [A.alloc([128, 4, TL], BF16) for _ in range(2)]
        bAP_ = [Buf(), Buf()]
        SGt = [A.alloc([128, TL], F32) for _ in range(2)]
        bSGt = [Buf(), Buf()]
        pend = {"fn": None, "n": 0, "m": 0}
        swiglu(s_, w_fg, w_fu, w_fd, 0, 0, None, None, AP_, bAP_, SGt, bSGt, None, None, pend)
        pend["fn"]()
        S.barrier()
        A.release(m)

    def moe_phase():
        s_ = 3
        norm_mod(s_, 4)
        m = A.mark()
        AP_ = [A.alloc([128, 4, TL], BF16) for _ in range(2)]
        bAP_ = [Buf(), Buf()]
        SGt = [A.alloc([128, TL], F32) for _ in range(2)]
        bSGt = [Buf(), Buf()]
        UGt = [A.alloc([128, TL], F32) for _ in range(2)]
        bUGt = [Buf(), Buf()]
        GTt = A.alloc([8, TB], F32); bGTt = Buf()
        Gb = [A.alloc([128, TB], F32) for _ in range(2)]
        bGb = [Buf(), Buf()]
        LG = A.alloc([128, 16, 8], F32); bLG = Buf()
        M8 = A.alloc([128, 16, 8], F32); bM8 = Buf()
        NM = A.alloc([128, 16], F32)
        EX = A.alloc([128, 16, 8], F32); bEX = Buf()
        DEN = A.alloc([128, 16], F32)
        for a in range(16):
            t = a // 4
            if a % 4 == 0:
                ps, bps = ps_next()
            for k in range(KC):
                S.op("pe", lambda h, ps=ps, a=a, k=k: h.matmul(
                    ps[:, (a % 4) * 8:(a % 4) * 8 + 8], hT[:, k, a * 128:(a + 1) * 128], WR[:, k, :],
                    start=(k == 0), stop=(k == KC - 1)), reads=[bhT[k][t], bWR], writes=[bps])
            if a % 4 == 3:
                S.op("dve", lambda h, ps=ps, t=t: h.tensor_copy(LG[:, t * 4:(t + 1) * 4, :],
                                                                ps[:, 0:32].rearrange("p (a e) -> p a e", a=4)),
                     reads=[bps], writes=[bLG])
        for a in range(16):
            S.op("dve", lambda h, a=a: h.max(out=M8[:, a, :], in_=LG[:, a, :]), reads=[bLG], writes=[bM8])
        S.op("dve", lambda h: h.tensor_scalar_mul(NM, M8[:, :, 0], -1.0), reads=[bM8], writes=[bM8])
        for a in range(16):
            S.op("act", lambda h, a=a: h.activation(out=EX[:, a, :], in_=LG[:, a, :], func=AF.Exp, bias=NM[:, a:a + 1], scale=1.0),
                 reads=[bLG, bM8], writes=[bEX])
        for a in range(16):
            S.op("dve", lambda h, a=a: h.tensor_single_scalar(LG[:, a, :], LG[:, a, :], M8[:, a, 1:2], op=ALU.is_ge),
                 reads=[bM8, bEX], writes=[bLG])
        S.op("dve", lambda h: h.tensor_mul(EX, EX, LG), reads=[bLG], writes=[bEX])
        S.op("dve", lambda h: h.reduce_sum(DEN, EX, axis=mybir.AxisListType.X), reads=[bEX], writes=[bM8])
        S.op("dve", lambda h: h.reciprocal(DEN, DEN), reads=[bM8], writes=[bM8])
        for a in range(16):
            S.op("dve", lambda h, a=a: h.tensor_scalar_mul(EX[:, a, :], EX[:, a, :], DEN[:, a:a + 1]),
                 reads=[bM8], writes=[bEX])
        for t in range(4):
            ps, bps = ps_next()
            for a4 in range(4):
                a = t * 4 + a4
                S.op("pe", lambda h, ps=ps, a=a, a4=a4: h.transpose(ps[0:8, a4 * 128:(a4 + 1) * 128], EX[:, a, :], IDN),
                     reads=[bEX, bCM], writes=[bps])
            S.op("act", lambda h, ps=ps, t=t: h.copy(GTt[:, t * TL:(t + 1) * TL], ps[0:8, :]), reads=[bps], writes=[bGTt])
        pend = {"fn": None, "n": 0, "m": 0}
        for e in range(NE):
            G, bG = Gb[e % 2], bGb[e % 2]
            for t in range(4):
                ps, bps = ps_next()
                S.op("pe", lambda h, ps=ps, e=e, t=t: h.matmul(ps, SEL[:, e * 128:(e + 1) * 128], GTt[:, t * TL:(t + 1) * TL],
                                                              start=True, stop=True), reads=[bSEL, bGTt], writes=[bps])
                S.op("act", lambda h, ps=ps, G=G, t=t: h.copy(G[:, t * TL:(t + 1) * TL], ps), reads=[bps], writes=[bG])
            swiglu(s_, w_mg, w_mu, w_md, e * D, e * FF, G, bG, AP_, bAP_, SGt, bSGt, UGt, bUGt, pend)
        pend["fn"]()
        S.barrier()
        A.release(m)

    def qkv_phase(blk_kv, blk_own, s0):
        s_ = 2
        norm_mod(s_, 4)
        m = A.mark()
        COS = A.alloc([128, TB], F32)
        SIN = A.alloc([128, TB], F32)
        bROPE = Buf()
        m1 = A.mark()
        HB = TB // 2
        PI_ = A.alloc([128, HB], I32)
        U = A.alloc([128, HB], F32)
        KI = A.alloc([128, HB], I32)
        KF = A.alloc([128, HB], F32)
        W2 = A.alloc([128, HB], F32)
        bT = gbuf("ropeT")
        for hb in range(2):
            c0 = hb * HB
            S.dma("sp", lambda h, c0=c0: h.dma_start(out=PI_, in_=poss[:, s0 + c0:s0 + c0 + HB].partition_broadcast(128)), writes=[bT])
            S.op("dve", lambda h: h.tensor_copy(U, PI_), reads=[bT], writes=[bT])
            S.op("dve", lambda h: h.tensor_scalar_mul(U, U, COLS[:, C_IF:C_IF + 1]), reads=[bT, bCOLS], writes=[bT])
            S.op("dve", lambda h: h.tensor_scalar_mul(U, U, 1.0 / TWO_PI), reads=[bT], writes=[bT])

            def frac_to(dst_scale_col, OUT, shift):
                S.op("dve", lambda h: h.tensor_scalar_add(W2, U, float(shift)), reads=[bT], writes=[bT])
                S.op("dve", lambda h: h.tensor_copy(KI, W2), reads=[bT], writes=[bT])
                S.op("dve", lambda h: h.tensor_copy(KF, KI), reads=[bT], writes=[bT])
                S.op("dve", lambda h: h.tensor_sub(W2, W2, KF), reads=[bT], writes=[bT])
                S.op("dve", lambda h: h.tensor_single_scalar(KF, W2, 0.5, op=ALU.is_gt), reads=[bT], writes=[bT])
                S.op("dve", lambda h: h.tensor_sub(W2, W2, KF), reads=[bT], writes=[bT])
                S.op("dve", lambda h: h.tensor_single_scalar(KF, W2, -0.5, op=ALU.is_lt), reads=[bT], writes=[bT])
                S.op("dve", lambda h: h.tensor_add(W2, W2, KF), reads=[bT], writes=[bT])
                S.op("act", lambda h: h.activation(out=OUT, in_=W2, func=AF.Sin, scale=TWO_PI), reads=[bT], writes=[bROPE])
                if dst_scale_col is not None:
                    S.op("dve", lambda h: h.tensor_scalar_mul(OUT, OUT, dst_scale_col), reads=[bROPE, bCOLS], writes=[bROPE])
            frac_to(COLS[:, C_SG:C_SG + 1], SIN[:, c0:c0 + HB], 0.0)
            frac_to(None, COS[:, c0:c0 + HB], 0.25)
        S.barrier()
        A.release(m1)
        T1 = [A.alloc([128, TL], F32) for _ in range(2)]
        bT1 = [Buf(), Buf()]
        T2 = [A.alloc([128, TL], F32) for _ in range(2)]
        bT2 = [Buf(), Buf()]
        OB = [A.alloc([128, TL], BF16) for _ in range(4)]
        bOB = [gbuf("ob%d" % i) for i in range(4)]
        cnt = 0
        ocnt = 0
        for c in range(KC):
            slot, bslot = ring_next()
            WQ = slot[:, 0:4 * KC * 128].rearrange("p (w k n) -> p w k n", w=4, k=KC)
            srcs = [w_q, w_qs, w_k, w_ks]
            wlist = [0, 1, 2, 3] if blk_own is not None else [2, 3]
            for w in wlist:
                wload(WQ[:, w], kview(srcs[w], c * 128, (c + 1) * 128), bslot)
            for t in range(4):
                for qk in ((0, 1) if blk_own is not None else (1,)):
                    psa, bpsa = ps_next()
                    psb, bpsb = ps_next()
                    for (ps, w) in ((psa, qk * 2), (psb, qk * 2 + 1)):
                        for k in range(KC):
                            S.op("pe", lambda h, ps=ps, w=w, k=k: h.matmul(
                                ps, WQ[:, w, k, :], hT[:, k, t * TL:(t + 1) * TL], start=(k == 0), stop=(k == KC - 1)),
                                reads=[bslot, bhT[k][t]], writes=[bpsa if ps is psa else bpsb])
                    t1, bt1 = T1[cnt % 2], bT1[cnt % 2]
                    t2, bt2 = T2[cnt % 2], bT2[cnt % 2]
                    cnt += 1
                    ob, bob = OB[ocnt % 4], bOB[ocnt % 4]
                    ocnt += 1
                    S.op("dve", lambda h, t1=t1, psa=psa: h.tensor_tensor(t1, psa, COS[:, t * TL:(t + 1) * TL], op=ALU.mult),
                         reads=[bpsa, bROPE], writes=[bt1])
                    S.op("dve", lambda h, t2=t2, psb=psb: h.tensor_tensor(t2, psb, SIN[:, t * TL:(t + 1) * TL], op=ALU.mult),
                         reads=[bpsb, bROPE], writes=[bt2])
                    S.op("dve", lambda h, ob=ob, t1=t1, t2=t2: h.tensor_tensor(ob, t1, t2, op=ALU.add),
                         reads=[bt1, bt2], writes=[bob])
                    if qk == 0:
                        col0 = blk_own * TB + t * TL
                        dst = QT[c * 128:(c + 1) * 128, col0:col0 + TL]
                        if DBG.get("noqstore"):
                            continue
                        S.dma("sp", lambda h, dst=dst, ob=ob: h.dma_start(out=dst, in_=ob), reads=[bob], writes=[bQT[c][blk_own]])
                    else:
                        col0 = blk_kv * TB + t * TL
                        dst = KT[c * 128:(c + 1) * 128, col0:col0 + TL]
                        S.dma("sp", lambda h, dst=dst, ob=ob: h.dma_start(out=dst, in_=ob), reads=[bob], writes=[bKT[c][blk_kv]])
        for half in range(2):
            slot, bslot = ring_next()
            WV = slot[:, 0:KC * 512].rearrange("p (k n) -> p k n", k=KC)
            wload(WV, kview(w_v, half * 512, (half + 1) * 512), bslot)
            for a in range(16):
                t = a // 4
                ps, bps = ps_next()
                for k in range(KC):
                    S.op("pe", lambda h, ps=ps, k=k, a=a: h.matmul(
                        ps, hT[:, k, a * 128:(a + 1) * 128], WV[:, k, :], start=(k == 0), stop=(k == KC - 1)),
                        reads=[bslot, bhT[k][t]], writes=[bps])
                ob, bob = OB[ocnt % 4], bOB[ocnt % 4]
                ocnt += 1
                S.op("act", lambda h, ob=ob, ps=ps: h.copy(ob, ps), reads=[bps], writes=[bob])
                r0 = blk_kv * TB + a * 128
                dst = VV[r0:r0 + 128, half * 512:(half + 1) * 512]
                S.dma("sp", lambda h, dst=dst, ob=ob: h.dma_start(out=dst, in_=ob), reads=[bob], writes=[bVV[blk_kv]])
        S.barrier()
        A.release(m)

    def attn_phase(blk_own):
        blk_kv = blk_own + 1
        s_ = 2
        m = A.mark()
        QH = [A.alloc([64, TB], BF16) for _ in range(2)]
        KH = [A.alloc([64, 2 * TB], BF16) for _ in range(2)]
        VH = [A.alloc([128, 32, 64], BF16) for _ in range(2)]
        bHD = [gbuf("hd0"), gbuf("hd1")]
        KD = A.alloc([64, 2 * TB], BF16); bKD = Buf()
        A2 = Arena(nc, hT_addr, hT_addr + KC * TB * 2)
        A2.n = 9000 + blk_own * 10
        OTs = A2.alloc([64, 3, TB], F32); bOTs = Buf()
        RSUM = A2.alloc([64, TB], F32); bRSUM = Buf()
        ATs = [A.alloc([64, TB], BF16) for _ in range(2)]
        bATs = [gbuf("ats0"), gbuf("ats1")]
        PT = [A.alloc([128, 256], BF16) for _ in range(3)]
        bPT = [Buf() for _ in range(3)]
        PM = [A.alloc([128, 256], BF16) for _ in range(3)]
        bPM = [Buf() for _ in range(3)]
        dil = (1, 4, 16)
        hcnt = 0
        pcnt = 0
        acnt = 0

        def load_head(hd, g, i):
            d = dil[g]
            r0 = hd * 64
            S.dma("sp", lambda h: h.dma_start(out=QH[i], in_=QT[r0:r0 + 64, blk_own * TB:(blk_own + 1) * TB]),
                  reads=[bQT[hd // 2][blk_own]], writes=[bHD[i]])
            S.dma("sp", lambda h: h.dma_start(out=KH[i], in_=KT[r0:r0 + 64, (blk_kv - 1) * TB:(blk_kv + 1) * TB]),
                  reads=[bKT[hd // 2][blk_kv - 1], bKT[hd // 2][blk_kv]], writes=[bHD[i]])
            nchunk = 2 * TB // (128 * d)
            vsrc = VV[(blk_kv - 1) * TB:(blk_kv + 1) * TB, r0:r0 + 64]
            if d == 1:
                src = vsrc.rearrange("(n i) c -> i n c", i=128)
                for q in range(4):
                    S.dma("sp", lambda h, q=q: h.dma_start(out=VH[i][:, q * 8:(q + 1) * 8, :], in_=src[:, q * 8:(q + 1) * 8, :]),
                          reads=[bVV[blk_kv - 1], bVV[blk_kv]], writes=[bHD[i]])
            else:
                src = vsrc.rearrange("(n i r) c -> n i r c", i=128, r=d)
                for n_ in range(nchunk):
                    S.dma("sp", lambda h, n_=n_: h.dma_start(out=VH[i][:, n_ * d:(n_ + 1) * d, :], in_=src[n_]),
                          reads=[bVV[blk_kv - 1], bVV[blk_kv]], writes=[bHD[i]])

        order = [(hh, g) for hh in range(5) for g in range(3)]
        load_head(order[0][1] * 5 + order[0][0], order[0][1], 0)
        for oi, (hh, g) in enumerate(order):
            if stop_after is not None and stop_after == "A_heads%d" % oi and blk_own == 0:
                S.barrier()
                stage(stop_after)
            hd = g * 5 + hh
            d = dil[g]
            i = oi % 2
            if oi + 1 < len(order):
                hh2, g2 = order[oi + 1]
                load_head(g2 * 5 + hh2, g2, (oi + 1) % 2)
            qh, kh, vh, bhd = QH[i], KH[i], VH[i], bHD[i]
            if d > 1:
                S.op("act", lambda h, kh=kh, d=d: h.copy(KD.rearrange("p (r i) -> p r i", r=d), kh.rearrange("p (i r) -> p r i", r=d)),
                     reads=[bhd], writes=[bKD])
                kst, bkst = KD, bKD
            else:
                kst, bkst = kh, bhd
            npair = 0 if DBG.get("nocomp") else 16

            def stageA(pr):
                    nonlocal pcnt
                    n, r = pr // d, pr % d
                    sl = lambda st0: slice(st0, st0 + 127 * d + 1, d)
                    qsl = sl(n * 128 * d + r)
                    ncls = 2 * TB // d
                    i0 = TB // d + 128 * n
                    if d > 1:
                        kcur = slice(r * ncls + i0, r * ncls + i0 + 128)
                        kprv = slice(r * ncls + i0 - 128, r * ncls + i0)
                    else:
                        kcur = slice(TB + n * 128, TB + n * 128 + 128)
                        kprv = slice(TB + (n - 1) * 128, TB + n * 128)
                    set_cur = 16 + n * d + r
                    set_prv = set_cur - d
                    pss, bpss = ps_next()
                    S.op("pe", lambda h, pss=pss, kst=kst, qh=qh, kprv=kprv, qsl=qsl: h.matmul(
                        pss[:, 0:128], kst[:, kprv], qh[:, qsl], start=True, stop=True), reads=[bhd, bkst], writes=[bpss])
                    S.op("pe", lambda h, pss=pss, kst=kst, qh=qh, kcur=kcur, qsl=qsl: h.matmul(
                        pss[:, 128:256], kst[:, kcur], qh[:, qsl], start=True, stop=True), reads=[bhd, bkst], writes=[bpss])
                    pt, bpt = PT[pcnt % 3], bPT[pcnt % 3]
                    pm, bpm = PM[pcnt % 3], bPM[pcnt % 3]
                    pcnt += 1
                    S.op("act", lambda h, pt=pt, pss=pss: h.activation(out=pt, in_=pss[:, 0:256], func=AF.Exp, scale=0.125),
                         reads=[bpss], writes=[bpt])
                    mk = MASKH if (blk_own == 0 and n == 0) else MASK
                    S.op("dve", lambda h, pm=pm, pt=pt, mk=mk: h.tensor_tensor(pm, pt, mk, op=ALU.mult),
                         reads=[bpt, bMASK], writes=[bpm])
                    return dict(n=n, r=r, qsl=qsl, set_cur=set_cur, set_prv=set_prv, pm=pm, bpm=bpm)

            def stageB(cx):
                    qsl, set_cur, set_prv, pm, bpm = cx['qsl'], cx['set_cur'], cx['set_prv'], cx['pm'], cx['bpm']
                    pso, bpso = ps_next()
                    S.op("pe", lambda h, pso=pso, vh=vh, pm=pm, set_prv=set_prv: h.matmul(
                        pso[0:64, 0:128], vh[:, set_prv, :], pm[:, 0:128], start=True, stop=False), reads=[bhd, bpm], writes=[bpso])
                    S.op("pe", lambda h, pso=pso, vh=vh, pm=pm, set_cur=set_cur: h.matmul(
                        pso[0:64, 0:128], vh[:, set_cur, :], pm[:, 128:256], start=False, stop=True), reads=[bhd, bpm], writes=[bpso])
                    S.op("pe", lambda h, pso=pso, pm=pm: h.matmul(
                        pso[0:64, 128:256], ONESB, pm[:, 0:128], start=True, stop=False), reads=[bONES, bpm], writes=[bpso])
                    S.op("pe", lambda h, pso=pso, pm=pm: h.matmul(
                        pso[0:64, 128:256], ONESB, pm[:, 128:256], start=False, stop=True), reads=[bONES, bpm], writes=[bpso])
                    S.op("act", lambda h, pso=pso, g=g, qsl=qsl: h.copy(OTs[:, g, qsl], pso[0:64, 0:128]), reads=[bpso], writes=[bOTs])
                    if g == 0:
                        S.op("dve", lambda h, pso=pso, qsl=qsl: h.tensor_copy(RSUM[:, qsl], pso[0:64, 128:256]),
                             reads=[bpso], writes=[bRSUM])
                    else:
                        S.op("dve", lambda h, pso=pso, qsl=qsl: h.tensor_tensor(RSUM[:, qsl], pso[0:64, 128:256], RSUM[:, qsl], op=ALU.add),
                             reads=[bpso], writes=[bRSUM])

            cxs = {}
            if npair:
                cxs[0] = stageA(0)
            for pr in range(npair):
                if pr + 1 < npair:
                    cxs[pr + 1] = stageA(pr + 1)
                stageB(cxs.pop(pr))
            if g == 2:
                S.op("dve", lambda h: h.reciprocal(RSUM, RSUM), reads=[bRSUM], writes=[bRSUM])
                for g3 in range(3):
                    ats, bats = ATs[acnt % 2], bATs[acnt % 2]
                    acnt += 1
                    S.op("dve", lambda h, ats=ats, g3=g3: h.tensor_tensor(ats, OTs[:, g3, :], RSUM, op=ALU.mult),
                         reads=[bOTs, bRSUM], writes=[bats])
                    hd3 = g3 * 5 + hh
                    dst = AT[hd3 * 64:(hd3 + 1) * 64, blk_own * TB:(blk_own + 1) * TB]
                    S.dma("sp", lambda h, dst=dst, ats=ats: h.dma_start(out=dst, in_=ats), reads=[bats], writes=[bAT[hd3][blk_own]])
        S.barrier()
        A.release(m)
        for c in range(KC):
            rows = 128 if c < 7 else 64
            src = AT[c * 128:c * 128 + rows, blk_own * TB:(blk_own + 1) * TB]
            deps = [bAT[2 * c][blk_own]] + ([bAT[2 * c + 1][blk_own]] if c < 7 else [])
            S.dma("sp", lambda h, c=c, rows=rows, src=src: h.dma_start(out=hT[0:rows, c, :], in_=src),
                  reads=deps, writes=[gbuf("atl")] + [bhT[c2][t2] for c2 in range(c + 1) for t2 in range(4)])
        for dg in range(2):
            slot, bslot = ring_next()
            WO_ = slot[:, 0:KC * 512].rearrange("p (k n) -> p k n", k=KC)
            wload(WO_, kview(w_o, dg * 512, (dg + 1) * 512), bslot)
            for t in range(4):
                for dl in range(4):
                    dch = dg * 4 + dl
                    ps, bps = ps_next()
                    for c in range(KC):
                        rows = 128 if c < 7 else 64
                        S.op("pe", lambda h, ps=ps, c=c, dl=dl, rows=rows: h.matmul(
                            ps, WO_[0:rows, c, dl * 128:(dl + 1) * 128], hT[0:rows, c, t * TL:(t + 1) * TL],
                            start=(c == 0), stop=(c == KC - 1)), reads=[bslot, bhT[c][t]], writes=[bps])
                    resid_add(ps, bps, s_, dch, t)
        S.barrier()

    def final_phase(blk_own):
        m = A.mark()
        SQ = [A.alloc([128, TL], F32) for _ in range(2)]
        bSQ = [Buf(), Buf()]
        RS = [A.alloc([128, TL], F32) for _ in range(2)]
        bRS = [Buf(), Buf()]
        YT = [A.alloc([128, KC, TL], F32) for _ in range(2)]
        bYT = [Buf(), Buf()]
        OS = [A.alloc([128, D], F32) for _ in range(3)]
        bOS = [gbuf("os%d" % i) for i in range(3)]
        oc = 0
        outs = []
        for t in range(4):
            rs, brs = RS[t % 2], bRS[t % 2]
            yt, byt = YT[t % 2], bYT[t % 2]
            norm_stats(t, SQ, bSQ, rs, brs)
            for k in range(KC):
                S.op("dve", lambda h, yt=yt, k=k, rs=rs: h.scalar_tensor_tensor(
                    out=yt[:, k, :], in0=xT[:, k, t * TL:(t + 1) * TL], scalar=COLS[:, C_FG + k:C_FG + k + 1], in1=rs,
                    op0=ALU.mult, op1=ALU.mult), reads=[bxT[k][t], brs, bCOLS], writes=[byt])
            for a in range(4):
                os_, bos = OS[oc % 3], bOS[oc % 3]
                oc += 1
                for half in range(2):
                    ps, bps = ps_next()
                    for kk in range(4):
                        k = half * 4 + kk
                        S.op("pe", lambda h, ps=ps, yt=yt, k=k, kk=kk, a=a: h.transpose(
                            ps[:, kk * 128:(kk + 1) * 128], yt[:, k, a * 128:(a + 1) * 128], IDN),
                            reads=[byt, bCM], writes=[bps])
                    if half == 0:
                        S.op("act", lambda h, ps=ps, os_=os_: h.copy(os_[:, 0:512], ps), reads=[bps], writes=[bos])
                    else:
                        S.op("dve", lambda h, ps=ps, os_=os_: h.tensor_copy(os_[:, 512:1024], ps), reads=[bps], writes=[bos])
                r0 = blk_own * TB + t * TL + a * 128
                S.dma("sp", lambda h, os_=os_, r0=r0: h.dma_start(out=out[r0:r0 + 128, :], in_=os_), reads=[bos])
                outs.append(bos)
        S.final_wait("sp", outs)
        S.barrier()
        A.release(m)

    class Stop(Exception):
        pass

    def stage(name):
        if stop_after == name:
            dbg = nc.dram_tensor("dbg", [D, TB], F32, kind="ExternalOutput").ap()
            bd = Buf()
            S.dma("sp", lambda h: h.dma_start(out=dbg.rearrange("(k p) t -> p k t", p=128), in_=xT),
                  reads=[b_ for row in bxT for b_ in row], writes=[bd])
            S.final_wait("sp", [bd])
            raise Stop()

    try:
        load_x(0, 1)
        norm_mod(0, 1)
        conv_phase(1, True, False)
        for blk in range(3):
            bn = "HAB"[blk]
            s0 = 512 + blk * TB
            load_x(s0, 4)
            stage(bn + "_load")
            norm_mod(0, 4)
            if stop_after == bn + "_norm":
                for k in range(KC):
                    for t in range(4):
                        S.op("dve", lambda h, k=k, t=t: h.tensor_copy(xT[:, k, t * TL:(t + 1) * TL], hT[:, k, t * TL:(t + 1) * TL]),
                             reads=[bhT[k][t]], writes=[bxT[k][t]])
                S.op("dve", lambda h: h.tensor_copy(xT[:, 0, 0:96], MODC), reads=[bMODC], writes=[bxT[0][0]])
                S.op("dve", lambda h: h.tensor_copy(xT[:, 0, 96:128], GS), reads=[bMODC], writes=[bxT[0][0]])
                stage(bn + "_norm")
            conv_phase(4, False, blk == 1)
            stage(bn + "_conv")
            ffn_phase()
            stage(bn + "_ffn")
            qkv_phase(blk, None if blk == 0 else blk - 1, s0)
            stage(bn + "_kv")
            if blk == 0:
                continue
            attn_phase(blk - 1)
            stage(bn + "_attn")
            moe_phase()
            stage(bn + "_moe")
            final_phase(blk - 1)
    except Stop:
        pass
    stats = S.emit(st)
    return nc, stats, st, used


_CACHE = {}
DBG = {}


def _prep_inputs(x, c, positions, mod_w, mod_b, norm_g, conv_w_in, conv_w, conv_w_out,
                 ffn_w_gate, ffn_w_up, ffn_w_down, attn_w_qkv, attn_w_o, router_w,
                 moe_w_gate, moe_w_up, moe_w_down, final_g):
    f32 = np.float32
    x = np.asarray(x, f32); c = np.asarray(c, f32); positions = np.asarray(positions, np.int32)

    def colmajor(v):
        v = np.asarray(v, f32)
        return np.ascontiguousarray(v.reshape(-1, 128).T)

    wqkv = np.asarray(attn_w_qkv, f32)[0]
    AW = 960

    def pad_cols(w):
        o = np.zeros((D, D), f32)
        o[:, :AW] = w
        return o

    def swap_halves(w):
        w4 = w.reshape(D, 15, 2, 32)
        return np.ascontiguousarray(w4[:, :, ::-1, :]).reshape(D, AW)
    wq, wk, wv = wqkv[:, 0:AW], wqkv[:, AW:2 * AW], wqkv[:, 2 * AW:3 * AW]
    wo = np.zeros((D, D), f32)
    wo[:AW, :] = np.asarray(attn_w_o, f32)[0]
    shared = {
        "modw": np.ascontiguousarray(np.asarray(mod_w, f32).reshape(4 * D, 3 * D)),
        "w_cin": np.ascontiguousarray(np.asarray(conv_w_in, f32)[0]),
        "w_cout": np.ascontiguousarray(np.asarray(conv_w_out, f32)[0]),
        "w_fg": np.ascontiguousarray(np.asarray(ffn_w_gate, f32)[0]),
        "w_fu": np.ascontiguousarray(np.asarray(ffn_w_up, f32)[0]),
        "w_fd": np.ascontiguousarray(np.asarray(ffn_w_down, f32)[0]),
        "w_q": pad_cols(wq), "w_qs": pad_cols(swap_halves(wq)),
        "w_k": pad_cols(wk), "w_ks": pad_cols(swap_halves(wk)),
        "w_v": pad_cols(wv), "w_o": wo,
        "w_r": np.ascontiguousarray(np.asarray(router_w, f32)[0]),
        "w_mg": np.ascontiguousarray(np.asarray(moe_w_gate, f32).reshape(NE * D, FF)),
        "w_mu": np.ascontiguousarray(np.asarray(moe_w_up, f32).reshape(NE * D, FF)),
        "w_md": np.ascontiguousarray(np.asarray(moe_w_down, f32).reshape(NE * FF, D)),
    }
    cm = np.zeros((128, 448), f32)
    cm[:, 0:128] = np.eye(128, dtype=f32)
    ik = np.arange(128)[:, None]
    aq = np.arange(128)[None, :]
    cm[:, 128:256] = (ik >= aq)
    cm[:, 256:384] = (ik <= aq)
    selm = np.zeros((8, NE * 128), f32)
    for e in range(NE):
        selm[e, e * 128:(e + 1) * 128] = 1.0
    shared["cmat"] = cm
    shared["sel"] = selm
    p = np.arange(128)
    inv_freq = (10000.0 ** (-(np.arange(0, 64, 2, dtype=np.float32)) / np.float32(64))).astype(f32)
    in_maps = []
    ng = np.asarray(norm_g, f32).reshape(4, D)
    mb = np.asarray(mod_b, f32).reshape(4, 3 * D)
    cw = np.asarray(conv_w, f32)[0]
    for core in range(8):
        b, half = core // 2, core % 2
        base = half * NOWN
        lo = base - 2560
        xsb = np.zeros((NS, D), f32)
        ps_ = np.zeros((1, NS), np.int32)
        v0 = max(lo, 0)
        xsb[v0 - lo:] = x[b, v0:base + NOWN]
        ps_[0, v0 - lo:] = positions[b, v0:base + NOWN]
        colsb = np.zeros((128, 176), f32)
        for s_ in range(4):
            colsb[:, s_ * 8:(s_ + 1) * 8] = colmajor(ng[s_])
        colsb[:, 32:40] = colmajor(final_g)
        for tap in range(3):
            colsb[:, 40 + tap * 8:48 + tap * 8] = colmajor(cw[tap])
        colsb[:, 64] = inv_freq[p % 32]
        colsb[:, 65] = np.where((p % 64) < 32, -1.0, 1.0)
        colsb[:, 66] = float(half)
        colsb[:, 67:75] = colmajor(c[b])
        for s_ in range(4):
            colsb[:, 75 + s_ * 24:75 + (s_ + 1) * 24] = colmajor(mb[s_])
        mp = dict(shared)
        mp["xs"] = xsb
        mp["poss"] = ps_
        mp["cols"] = colsb
        in_maps.append(mp)
    return in_maps


def kernel(**inputs):
    if "nc" not in _CACHE:
        nc, stats, st, used = build_program()
        _CACHE["nc"] = nc
        _CACHE["st"] = st
        _CACHE["used"] = used
    nc = _CACHE["nc"]
    in_maps = _prep_inputs(**inputs)
    in_maps = [{k: v for k, v in m.items() if k in _CACHE["used"]} for m in in_maps]
    res = run_bass_kernel_spmd(nc, in_maps, core_ids=list(range(8)))
    outp = np.zeros((4, 8192, D), np.float32)
    for core in range(8):
        b, half = core // 2, core % 2
        outp[b, half * NOWN:(half + 1) * NOWN] = res.results[core]["out"]
    return outp
```

```python
import types
import numpy as np
from contextlib import ExitStack
import concourse.bass as bass
import concourse.mybir as mybir
from concourse.bass_utils import run_bass_kernel_spmd

F32 = mybir.dt.float32
BF16 = mybir.dt.bfloat16
I32 = mybir.dt.int32
AF = mybir.ActivationFunctionType
ALU = mybir.AluOpType

D = 1024
KC = 8
FF = 2816
FC = 22
NE = 8
TB = 2048
TL = 512
NS = 6656
NKV = 6144
NOWN = 4096
EPS = 1e-6
TWO_PI = float(2 * np.pi)


class Buf:
    __slots__ = ("name", "lw", "rde", "rdd", "dsem", "dcnt", "ps")

    def __init__(self, name=""):
        self.name = name
        self.lw = None
        self.rde = {}
        self.rdd = []
        self.dsem = None
        self.dcnt = 0
        self.ps = False


def _freeze(fn):
    if fn is None or fn.__closure__ is None:
        return fn
    cells = []
    for c in fn.__closure__:
        try:
            cells.append(types.CellType(c.cell_contents))
        except ValueError:
            cells.append(c)
    return types.FunctionType(fn.__code__, fn.__globals__, fn.__name__, fn.__defaults__, tuple(cells))


class Sched:
    ENGS = ("pe", "act", "dve", "pool", "sp")

    def __init__(self, nc):
        self.nc = nc
        self.ops = {e: [] for e in self.ENGS}
        self.seen = {e: {} for e in self.ENGS}
        self.ref = {e: set() for e in self.ENGS}
        self.ndsem = 0
        self.dma_tokens = []
        self.lastc = {e: 0 for e in self.ENGS}

    def _need(self, eng, tok, waits):
        if tok is None:
            return
        if tok[0] == 'e':
            _, pe_, idx = tok
            if pe_ == eng and eng == "pe":
                return
            if self.seen[eng].get(pe_, 0) >= idx:
                return
            self.seen[eng][pe_] = idx
            self.ref[pe_].add(idx)
            waits.append(tok)
        else:
            _, s, v = tok
            key = ('d', s)
            if self.seen[eng].get(key, 0) >= v:
                return
            self.seen[eng][key] = v
            waits.append(tok)

    def _hazards(self, eng, reads, writes):
        waits = []
        for b in reads:
            self._need(eng, b.lw, waits)
            if b.ps:
                for e2, i2 in b.rde.items():
                    if e2 != eng:
                        self._need(eng, ('e', e2, i2), waits)
        for b in writes:
            self._need(eng, b.lw, waits)
            for e2, i2 in b.rde.items():
                self._need(eng, ('e', e2, i2), waits)
            for t in b.rdd:
                self._need(eng, t, waits)
        return waits

    def op(self, eng, fn, reads=(), writes=()):
        waits = self._hazards(eng, reads, writes)
        idx = len(self.ops[eng]) + 1
        tok = ('e', eng, idx)
        self.ops[eng].append([waits, _freeze(fn), False, None])
        self.lastc[eng] = idx
        for b in reads:
            b.rde[eng] = idx
        for b in writes:
            b.lw = tok
            b.rde = {}
            b.rdd = []
        return tok

    def dma(self, eng, fn, reads=(), writes=()):
        waits = self._hazards(eng, reads, writes)
        owner = writes[0] if writes else reads[0]
        if owner.dsem is None:
            owner.dsem = self.ndsem
            self.ndsem += 1
        owner.dcnt += 16
        tok = ('d', owner.dsem, owner.dcnt)
        self.ops[eng].append([waits, _freeze(fn), True, owner.dsem])
        for b in reads:
            b.rdd.append(tok)
        for b in writes:
            b.lw = tok
            b.rde = {}
            b.rdd = []
        if eng == "sp":
            self.dma_tokens.append(tok)
        return tok

    def barrier(self, engs=("pe", "act", "dve", "sp")):
        last = {e: self.lastc[e] for e in ("pe", "act", "dve", "pool")}
        toks = self.dma_tokens
        self.dma_tokens = []
        for e in engs:
            waits = []
            for e2, i2 in last.items():
                if e2 != e and i2 > 0:
                    self._need(e, ('e', e2, i2), waits)
            for t in toks:
                self._need(e, t, waits)
            if waits:
                self.ops[e].append([waits, None, False, None])

    def final_wait(self, eng, bufs):
        waits = []
        for b in bufs:
            self._need(eng, b.lw, waits)
            for t in b.rdd:
                self._need(eng, t, waits)
        self.ops[eng].append([waits, None, False, None])

    def emit(self, stack):
        nc = self.nc
        print("ndsem", self.ndsem)
        esem = {e: stack.enter_context(nc.semaphore("es_" + e)) for e in self.ENGS}
        dsem = [stack.enter_context(nc.semaphore("ds%d" % i)) for i in range(self.ndsem)]
        cum = {}
        for e in self.ENGS:
            c = 0
            arr = [0]
            for i in range(1, len(self.ops[e]) + 1):
                if i in self.ref[e]:
                    c += 1
                arr.append(c)
            cum[e] = arr
        block = stack.enter_context(nc.Block())
        handles = {"pe": block.tensor, "act": block.scalar, "dve": block.vector,
                   "pool": block.gpsimd, "sp": block.sync}

        def mk(e):
            def body(h):
                for i, (waits, fn, is_dma, ds) in enumerate(self.ops[e], start=1):
                    for t in waits:
                        if t[0] == 'e':
                            h.wait_ge(esem[t[1]], cum[t[1]][t[2]])
                        else:
                            h.wait_ge(dsem[t[1]], t[2])
                    if fn is None:
                        continue
                    ins = fn(h)
                    if is_dma:
                        ins.then_inc(dsem[ds], 16)
                    elif i in self.ref[e]:
                        ins.then_inc(esem[e], 1)
            return body

        for e in self.ENGS:
            handles[e](mk(e))
        return {e: (len(self.ops[e]), cum[e][-1]) for e in self.ENGS}


class Arena:
    def __init__(self, nc, start, end):
        self.nc, self.ptr, self.end, self.n = nc, start, end, 0

    def alloc(self, shape, dt):
        es = 4 if dt in (F32, I32) else 2
        nbytes = int(np.prod(shape[1:])) * es
        addr = (self.ptr + 31) // 32 * 32
        assert addr + nbytes <= self.end, ("SBUF overflow", addr, nbytes, self.end)
        self.ptr = addr + nbytes
        self.n += 1
        return self.nc.alloc_sbuf_tensor_at("a%d" % self.n, list(shape), dt, offset=addr).ap()

    def mark(self):
        return self.ptr

    def release(self, m):
        self.ptr = m


def build_program(stop_after=None):
    nc = bass.Bass("TRN2", target_bir_lowering=False)

    used = []

    class Lazy:
        def __init__(self, name, shape, dt):
            self.a = (name, list(shape), dt)
            self.v = None

        def get(self):
            if self.v is None:
                self.v = nc.dram_tensor(self.a[0], self.a[1], self.a[2], kind="ExternalInput").ap()
                used.append(self.a[0])
            return self.v

        def __getitem__(self, key):
            return self.get()[key]

        def rearrange(self, *a, **k):
            return self.get().rearrange(*a, **k)

    def din(name, shape, dt=F32):
        return Lazy(name, shape, dt)

    xs = din("xs", [NS, D])
    poss = din("poss", [1, NS], I32)
    cols = din("cols", [128, 176])
    cmat = din("cmat", [128, 128 + 256 + 64])
    sel = din("sel", [8, NE * 128])
    modw = din("modw", [4 * D, 3 * D])
    w_cin = din("w_cin", [D, 3 * D])
    w_cout = din("w_cout", [D, D])
    w_fg = din("w_fg", [D, FF])
    w_fu = din("w_fu", [D, FF])
    w_fd = din("w_fd", [FF, D])
    w_q = din("w_q", [D, D])
    w_qs = din("w_qs", [D, D])
    w_k = din("w_k", [D, D])
    w_ks = din("w_ks", [D, D])
    w_v = din("w_v", [D, D])
    w_o = din("w_o", [D, D])
    w_r = din("w_r", [D, NE])
    w_mg = din("w_mg", [NE * D, FF])
    w_mu = din("w_mu", [NE * D, FF])
    w_md = din("w_md", [NE * FF, D])
    out = nc.dram_tensor("out", [NOWN, D], F32, kind="ExternalOutput").ap()
    PAD0 = nc.dram_tensor("PAD0", [128, 1024], F32, kind="Internal").ap()
    QKT = nc.dram_tensor("QKT", [3 * D, NKV], BF16, kind="Internal").ap()
    KT = QKT[0:D, :]
    QT = QKT[D:2 * D, 0:NOWN]
    VV = nc.dram_tensor("VV", [NKV, D], BF16, kind="Internal").ap()
    AT = QKT[2 * D:3 * D, 0:NOWN]
    _q1, _k1, _a1, _v1 = Buf(), Buf(), Buf(), Buf()
    _q = [_q1, _q1]
    _k = [_k1, _k1, _k1]
    _a = [_a1, _a1]
    bQT = [_q for _ in range(KC)]
    bKT = [_k for _ in range(KC)]
    bVV = [_v1, _v1, _v1]
    bAT = [_a for _ in range(16)]
    _gb = {}

    def gbuf(name):
        if name not in _gb:
            _gb[name] = Buf(name)
        return _gb[name]

    S = Sched(nc)
    st = ExitStack()
    A = Arena(nc, 16512, 229376 - 64)

    xT = A.alloc([128, KC, TB], F32)
    bxT = [[Buf() for _ in range(4)] for _ in range(KC)]
    hT_addr = (A.ptr + 31) // 32 * 32
    hT = A.alloc([128, KC, TB], BF16)
    bhT = [[Buf() for _ in range(4)] for _ in range(KC)]
    COLS = A.alloc([128, 176], F32); bCOLS = Buf()
    CM = A.alloc([128, 448], F32); bCM = Buf()
    IDN = CM[:, 0:128]
    ONES = A.alloc([128, 128], F32); bONES = Buf()
    ONESB = A.alloc([128, 64], BF16)
    MASK = A.alloc([128, 256], BF16); bMASK = Buf()
    MASKH = A.alloc([128, 256], BF16)
    SEL = A.alloc([8, NE * 128], F32); bSEL = Buf()
    MODC = A.alloc([128, 96], F32); bMODC = Buf()
    GS = A.alloc([128, 32], F32)
    WR = A.alloc([128, KC, NE], BF16); bWR = Buf()
    VCAR = A.alloc([128, KC, 2], F32); bVCAR = [Buf() for _ in range(KC)]
    C_NG, C_FG, C_CW, C_IF, C_SG, C_FL, C_C, C_MB = 0, 32, 40, 64, 65, 66, 67, 75
    EPSC = A.alloc([128, 1], F32)
    NSLOT = 2
    RING = [A.alloc([128, 12288], BF16) for _ in range(NSLOT)]
    bRING = [Buf() for _ in range(NSLOT)]
    ring_ctr = [0]

    def ring_next():
        i = ring_ctr[0] % NSLOT
        ring_ctr[0] += 1
        return RING[i], bRING[i]

    PS = [nc.alloc_psum_tensor("ps%d" % i, [128, 512], F32).ap() for i in range(8)]
    bPS = [Buf() for _ in range(8)]
    for b_ in bPS:
        b_.ps = True
    ps_ctr = [0]

    def ps_next():
        i = ps_ctr[0] % 8
        ps_ctr[0] += 1
        return PS[i], bPS[i]

    S.dma("sp", lambda h: h.dma_start(out=COLS, in_=cols[:, :]), writes=[bCOLS])
    S.dma("sp", lambda h: h.dma_start(out=CM, in_=cmat[:, :]), writes=[bCM])
    S.dma("sp", lambda h: h.dma_start(out=SEL, in_=sel[:, :]), writes=[bSEL])
    S.dma("sp", lambda h: h.dma_start(out=PAD0[:, 0:176], in_=COLS), reads=[bCOLS], writes=[Buf("pad0")])
    S.dma("pool", lambda h: h.dma_start(out=WR, in_=w_r.rearrange("(k p) e -> p k e", p=128)), writes=[bWR])
    S.op("dve", lambda h: h.memset(ONES, 1.0), writes=[bONES])
    S.op("dve", lambda h: h.memset(ONESB, 1.0), writes=[bONES])
    S.op("dve", lambda h: h.memset(EPSC, EPS), writes=[bONES])
    S.op("dve", lambda h: h.tensor_copy(MASK, CM[:, 128:384]), reads=[bCM], writes=[bMASK])
    S.op("dve", lambda h: h.tensor_copy(MASKH[:, 128:256], CM[:, 256:384]), reads=[bCM], writes=[bMASK])
    S.op("dve", lambda h: h.tensor_scalar_mul(MASKH[:, 0:128], CM[:, 128:256], COLS[:, C_FL:C_FL + 1]),
         reads=[bCM, bCOLS], writes=[bMASK])
    for k in range(KC):
        S.op("dve", lambda h, k=k: h.memset(VCAR[:, k, :], 0.0), writes=[bVCAR[k]])

    m0 = A.mark()
    SC = A.alloc([128, KC], F32); bSC = Buf()
    S.op("act", lambda h: h.activation(out=SC, in_=COLS[:, C_C:C_C + 8], func=AF.Silu), reads=[bCOLS], writes=[bSC])
    MW = [A.alloc([128, KC, 512], F32) for _ in range(2)]
    bMW = [Buf(), Buf()]
    psm, bpsm = ps_next()
    gi = 0
    for s_ in range(4):
        for cg in range(6):
            mw, bmw = MW[gi % 2], bMW[gi % 2]
            gi += 1
            src = modw[s_ * D:(s_ + 1) * D, cg * 512:(cg + 1) * 512].rearrange("(k p) n -> p k n", p=128)
            S.dma("sp", lambda h, mw=mw, src=src: h.dma_start(out=mw, in_=src), writes=[bmw])
            for jj in range(4):
                col = s_ * 24 + cg * 4 + jj
                for k in range(KC):
                    S.op("pe", lambda h, mw=mw, jj=jj, k=k, col=col: h.matmul(
                        psm[:, col:col + 1], mw[:, k, jj * 128:(jj + 1) * 128], SC[:, k:k + 1],
                        start=(k == 0), stop=(k == KC - 1)), reads=[bmw, bSC], writes=[bpsm])
    S.op("dve", lambda h: h.tensor_tensor(MODC, psm[:, 0:96], COLS[:, C_MB:C_MB + 96], op=ALU.add),
         reads=[bpsm, bCOLS], writes=[bMODC])
    for s_ in range(4):
        S.op("dve", lambda h, s_=s_: h.tensor_scalar_add(GS[:, s_ * 8:(s_ + 1) * 8], MODC[:, s_ * 24 + 8:s_ * 24 + 16], 1.0),
             reads=[bMODC], writes=[bMODC])
        S.op("dve", lambda h, s_=s_: h.tensor_mul(GS[:, s_ * 8:(s_ + 1) * 8], GS[:, s_ * 8:(s_ + 1) * 8],
                                                  COLS[:, C_NG + s_ * 8:C_NG + (s_ + 1) * 8]),
             reads=[bMODC, bCOLS], writes=[bMODC])
    S.barrier()
    A.release(m0)

    def shiftc(s_, k):
        return MODC[:, s_ * 24 + k:s_ * 24 + k + 1]

    def gatec(s_, k):
        return MODC[:, s_ * 24 + 16 + k:s_ * 24 + 16 + k + 1]

    def gsc(s_, k):
        return GS[:, s_ * 8 + k:s_ * 8 + k + 1]

    def load_x(s0, ntiles):
        m = A.mark()
        XL = [A.alloc([128, 4, D], F32) for _ in range(2)]
        bXL = [gbuf("xl0"), gbuf("xl1")]
        for t in range(ntiles):
            xl, bxl = XL[t % 2], bXL[t % 2]
            src = xs[s0 + t * TL:s0 + (t + 1) * TL, :].rearrange("(a p) f -> p a f", p=128)
            S.dma("sp", lambda h, xl=xl, src=src: h.dma_start(out=xl, in_=src), writes=[bxl])
            for k in range(KC):
                ps, bps = ps_next()
                for a in range(4):
                    S.op("pe", lambda h, ps=ps, xl=xl, a=a, k=k: h.transpose(
                        ps[:, a * 128:(a + 1) * 128], xl[:, a, k * 128:(k + 1) * 128], IDN),
                        reads=[bxl, bCM], writes=[bps])
                dst = xT[:, k, t * TL:(t + 1) * TL]
                if k % 2 == 0:
                    S.op("act", lambda h, ps=ps, dst=dst: h.copy(dst, ps), reads=[bps], writes=[bxT[k][t]])
                else:
                    S.op("dve", lambda h, ps=ps, dst=dst: h.tensor_copy(dst, ps), reads=[bps], writes=[bxT[k][t]])
        S.barrier()
        A.release(m)

    def norm_stats(t, SQ, bSQ, RS, bRS):
        ps, bps = ps_next()
        for k in range(KC):
            sq, bsq = SQ[k % 2], bSQ[k % 2]
            S.op("act", lambda h, sq=sq, k=k: h.activation(out=sq, in_=xT[:, k, t * TL:(t + 1) * TL], func=AF.Square),
                 reads=[bxT[k][t]], writes=[bsq])
            S.op("pe", lambda h, ps=ps, sq=sq, k=k: h.matmul(ps, ONES, sq, start=(k == 0), stop=(k == KC - 1)),
                 reads=[bsq, bONES], writes=[bps])
        S.op("act", lambda h, ps=ps: h.activation(out=RS, in_=ps, func=AF.Sqrt, bias=EPSC[:, 0:1], scale=1.0 / D),
             reads=[bps, bONES], writes=[bRS])
        S.op("dve", lambda h: h.reciprocal(RS, RS), reads=[bRS], writes=[bRS])

    def norm_mod(s_, ntiles, keep=False):
        m = A.mark()
        SQ = [A.alloc([128, TL], F32) for _ in range(2)]
        bSQ = [Buf(), Buf()]
        RS = [A.alloc([128, TL], F32) for _ in range(2)]
        bRS = [Buf(), Buf()]
        TM = [A.alloc([128, TL], F32) for _ in range(2)]
        bTM = [Buf(), Buf()]
        for t in range(ntiles):
            rs, brs = RS[t % 2], bRS[t % 2]
            norm_stats(t, SQ, bSQ, rs, brs)
            for k in range(KC):
                tm, btm = TM[k % 2], bTM[k % 2]
                S.op("dve", lambda h, tm=tm, k=k, rs=rs: h.scalar_tensor_tensor(
                    out=tm, in0=xT[:, k, t * TL:(t + 1) * TL], scalar=gsc(s_, k), in1=rs, op0=ALU.mult, op1=ALU.mult),
                    reads=[bxT[k][t], brs, bMODC], writes=[btm])
                S.op("act", lambda h, tm=tm, k=k: h.activation(
                    out=hT[:, k, t * TL:(t + 1) * TL], in_=tm, func=AF.Identity, bias=shiftc(s_, k), scale=1.0),
                    reads=[btm, bMODC], writes=[bhT[k][t]])
        if keep:
            return
        S.barrier()
        A.release(m)

    def wload(dst, src, bslot):
        S.dma("pool", lambda h: h.dma_start(out=dst, in_=src), writes=[bslot])

    def kview(w, c0, c1):
        return w[:, c0:c1].rearrange("(k p) n -> p k n", p=128)

    def resid_add(ps, bps, s_, dch, t):
        dst = xT[:, dch, t * TL:(t + 1) * TL]
        S.op("dve", lambda h: h.scalar_tensor_tensor(out=dst, in0=ps, scalar=gatec(s_, dch), in1=dst,
                                                     op0=ALU.mult, op1=ALU.add),
             reads=[bps, bMODC], writes=[bxT[dch][t]])

    def conv_phase(ntiles, v_only, first_flag):
        s_ = 0
        m = A.mark()
        VB = [A.alloc([128, TL + 2], F32) for _ in range(2)]
        bVB = [Buf(), Buf()]
        CG = [A.alloc([128, TL], F32) for _ in range(2)]
        bCG = [Buf(), Buf()]
        AC = [A.alloc([128, TL], F32) for _ in range(2)]
        bAC = [Buf(), Buf()]
        MT = [A.alloc([128, 2, TL], BF16) for _ in range(2)]
        bMT = [Buf(), Buf()]
        if first_flag:
            for k in range(KC):
                S.op("dve", lambda h, k=k: h.tensor_scalar_mul(VCAR[:, k, :], VCAR[:, k, :], COLS[:, C_FL:C_FL + 1]),
                     reads=[bCOLS], writes=[bVCAR[k]])
        cnt = 0
        for jg in range(4):
            slot, bslot = ring_next()
            WI = slot[:, 0:KC * 768].rearrange("p (k n) -> p k n", k=KC)
            WO = slot[:, 6144:6144 + 2 * D].rearrange("p (j n) -> p j n", j=2)
            for jl in range(2):
                j = jg * 2 + jl
                for part in range(3):
                    wload(WI[:, :, (jl * 3 + part) * 128:(jl * 3 + part + 1) * 128],
                          kview(w_cin, part * D + j * 128, part * D + (j + 1) * 128), bslot)
            if not v_only:
                wload(WO, w_cout[jg * 256:(jg + 1) * 256, :].rearrange("(j p) n -> p j n", p=128), bslot)
            for t in range(ntiles):
                mt, bmt = MT[(jg * ntiles + t) % 2], bMT[(jg * ntiles + t) % 2]
                for jl in range(2):
                    j = jg * 2 + jl
                    vb, bvb = VB[cnt % 2], bVB[cnt % 2]
                    cgb, bcgb = CG[cnt % 2], bCG[cnt % 2]
                    ac, bac = AC[cnt % 2], bAC[cnt % 2]
                    cnt += 1
                    pss = []
                    for part in range(3):
                        if v_only and part == 0:
                            pss.append((None, None))
                            continue
                        ps, bps = ps_next()
                        for k in range(KC):
                            S.op("pe", lambda h, ps=ps, k=k, part=part, jl=jl: h.matmul(
                                ps, WI[:, k, (jl * 3 + part) * 128:(jl * 3 + part + 1) * 128], hT[:, k, t * TL:(t + 1) * TL],
                                start=(k == 0), stop=(k == KC - 1)), reads=[bslot, bhT[k][t]], writes=[bps])
                        pss.append((ps, bps))
                    (psb, bpsb), (psc, bpsc), (psu, bpsu) = pss
                    S.op("act", lambda h, cgb=cgb, psc=psc: h.copy(cgb, psc), reads=[bpsc], writes=[bcgb])
                    S.op("dve", lambda h, vb=vb, j=j: h.tensor_copy(vb[:, 0:2], VCAR[:, j, :]), reads=[bVCAR[j]], writes=[bvb])
                    S.op("dve", lambda h, vb=vb, cgb=cgb, psu=psu: h.tensor_tensor(vb[:, 2:TL + 2], psu, cgb, op=ALU.mult),
                         reads=[bpsu, bcgb], writes=[bvb])
                    S.op("dve", lambda h, vb=vb, j=j: h.tensor_copy(VCAR[:, j, :], vb[:, TL:TL + 2]), reads=[bvb], writes=[bVCAR[j]])
                    if v_only:
                        continue
                    cw = lambda tap, j=j: COLS[:, C_CW + tap * 8 + j:C_CW + tap * 8 + j + 1]
                    S.op("dve", lambda h, ac=ac, vb=vb, cw=cw: h.tensor_scalar_mul(ac, vb[:, 2:TL + 2], cw(2)),
                         reads=[bvb, bCOLS], writes=[bac])
                    S.op("dve", lambda h, ac=ac, vb=vb, cw=cw: h.scalar_tensor_tensor(
                        out=ac, in0=vb[:, 1:TL + 1], scalar=cw(1), in1=ac, op0=ALU.mult, op1=ALU.add),
                        reads=[bvb, bCOLS], writes=[bac])
                    S.op("dve", lambda h, ac=ac, vb=vb, cw=cw: h.scalar_tensor_tensor(
                        out=ac, in0=vb[:, 0:TL], scalar=cw(0), in1=ac, op0=ALU.mult, op1=ALU.add),
                        reads=[bvb, bCOLS], writes=[bac])
                    S.op("dve", lambda h, mt=mt, jl=jl, psb=psb, ac=ac: h.tensor_tensor(mt[:, jl, :], psb, ac, op=ALU.mult),
                         reads=[bpsb, bac], writes=[bmt])
                if v_only:
                    continue
                for dch in range(KC):
                    ps, bps = ps_next()
                    for jl in range(2):
                        S.op("pe", lambda h, ps=ps, jl=jl, dch=dch, mt=mt: h.matmul(
                            ps, WO[:, jl, dch * 128:(dch + 1) * 128], mt[:, jl, :], start=(jl == 0), stop=(jl == 1)),
                            reads=[bslot, bmt], writes=[bps])
                    resid_add(ps, bps, s_, dch, t)
        S.barrier()
        A.release(m)

    def swiglu(s_, wg, wu, wd, row0g, row0d, G, bG, AP_, bAP_, SGt, bSGt, UGt, bUGt, pend):
        groups = [(0, 4), (4, 4), (8, 4), (12, 4), (16, 4), (20, 2)]
        for (f0, fg) in groups:
            slot, bslot = ring_next()
            WG = slot[:, 0:KC * fg * 128].rearrange("p (k n) -> p k n", k=KC)
            WU = slot[:, 4096:4096 + KC * fg * 128].rearrange("p (k n) -> p k n", k=KC)
            WD = slot[:, 8192:8192 + fg * D].rearrange("p (f n) -> p f n", f=fg)
            wload(WG, wg[row0g:row0g + D, f0 * 128:(f0 + fg) * 128].rearrange("(k p) n -> p k n", p=128), bslot)
            wload(WU, wu[row0g:row0g + D, f0 * 128:(f0 + fg) * 128].rearrange("(k p) n -> p k n", p=128), bslot)
            wload(WD, wd[row0d + f0 * 128:row0d + (f0 + fg) * 128, :].rearrange("(f p) n -> p f n", p=128), bslot)
            for t in range(4):
                ap_, bap_ = AP_[pend["n"] % 2], bAP_[pend["n"] % 2]
                pend["n"] += 1
                for fi in range(fg):
                    psg, bpsg = ps_next()
                    for k in range(KC):
                        S.op("pe", lambda h, psg=psg, k=k, fi=fi, WG=WG: h.matmul(
                            psg, WG[:, k, fi * 128:(fi + 1) * 128], hT[:, k, t * TL:(t + 1) * TL],
                            start=(k == 0), stop=(k == KC - 1)), reads=[bslot, bhT[k][t]], writes=[bpsg])
                    psu, bpsu = ps_next()
                    for k in range(KC):
                        S.op("pe", lambda h, psu=psu, k=k, fi=fi, WU=WU: h.matmul(
                            psu, WU[:, k, fi * 128:(fi + 1) * 128], hT[:, k, t * TL:(t + 1) * TL],
                            start=(k == 0), stop=(k == KC - 1)), reads=[bslot, bhT[k][t]], writes=[bpsu])
                    i2 = pend["m"] % 2
                    pend["m"] += 1
                    sg, bsg = SGt[i2], bSGt[i2]
                    S.op("act", lambda h, sg=sg, psg=psg: h.activation(out=sg, in_=psg, func=AF.Silu),
                         reads=[bpsg], writes=[bsg])
                    if G is not None:
                        ug, bug = UGt[i2], bUGt[i2]
                        S.op("dve", lambda h, ug=ug, psu=psu: h.tensor_tensor(ug, psu, G[:, t * TL:(t + 1) * TL], op=ALU.mult),
                             reads=[bpsu, bG], writes=[bug])
                        S.op("dve", lambda h, ap_=ap_, fi=fi, sg=sg, ug=ug: h.tensor_tensor(ap_[:, fi, :], sg, ug, op=ALU.mult),
                             reads=[bsg, bug], writes=[bap_])
                    else:
                        S.op("dve", lambda h, ap_=ap_, fi=fi, sg=sg, psu=psu: h.tensor_tensor(ap_[:, fi, :], psu, sg, op=ALU.mult),
                             reads=[bpsu, bsg], writes=[bap_])
                if pend["fn"] is not None:
                    pend["fn"]()

                def ydown(t=t, fg=fg, WD=WD, ap_=ap_, bap_=bap_, bslot=bslot):
                    for dch in range(KC):
                        ps, bps = ps_next()
                        for fi in range(fg):
                            S.op("pe", lambda h, ps=ps, fi=fi, dch=dch: h.matmul(
                                ps, WD[:, fi, dch * 128:(dch + 1) * 128], ap_[:, fi, :], start=(fi == 0), stop=(fi == fg - 1)),
                                reads=[bslot, bap_], writes=[bps])
                        resid_add(ps, bps, s_, dch, t)
                pend["fn"] = ydown

    def ffn_phase():
        s_ = 1
        m = A.mark()
        norm_mod(s_, 4, keep=True)
        AP_ = [A.alloc([128, 4, TL], BF16) for _ in range(2)]
        bAP_ = [Buf(), Buf()]
        SGt = [A.alloc([128, TL], F32) for _ in range(2)]
        bSGt = [Buf(), Buf()]
        pend = {"fn": None, "n": 0, "m": 0}
        swiglu(s_, w_fg, w_fu, w_fd, 0, 0, None, None, AP_, bAP_, SGt, bSGt, None, None, pend)
        pend["fn"]()
        S.barrier()
        A.release(m)

    def moe_phase():
        s_ = 3
        m = A.mark()
        norm_mod(s_, 4, keep=True)
        AP_ = [A.alloc([128, 4, TL], BF16) for _ in range(2)]
        bAP_ = [Buf(), Buf()]
        SGt = [A.alloc([128, TL], F32) for _ in range(2)]
        bSGt = [Buf(), Buf()]
        UGt = [A.alloc([128, TL], F32) for _ in range(2)]
        bUGt = [Buf(), Buf()]
        GTt = A.alloc([8, TB], F32); bGTt = Buf()
        Gb = [A.alloc([128, TB], F32) for _ in range(2)]
        bGb = [Buf(), Buf()]
        LG = A.alloc([128, 16, 8], F32); bLG = Buf()
        M8 = A.alloc([128, 16, 8], F32); bM8 = Buf()
        NM = A.alloc([128, 16], F32)
        EX = A.alloc([128, 16, 8], F32); bEX = Buf()
        DEN = A.alloc([128, 16], F32)
        for a in range(16):
            t = a // 4
            if a % 4 == 0:
                ps, bps = ps_next()
            for k in range(KC):
                S.op("pe", lambda h, ps=ps, a=a, k=k: h.matmul(
                    ps[:, (a % 4) * 8:(a % 4) * 8 + 8], hT[:, k, a * 128:(a + 1) * 128], WR[:, k, :],
                    start=(k == 0), stop=(k == KC - 1)), reads=[bhT[k][t], bWR], writes=[bps])
            if a % 4 == 3:
                S.op("dve", lambda h, ps=ps, t=t: h.tensor_copy(LG[:, t * 4:(t + 1) * 4, :],
                                                                ps[:, 0:32].rearrange("p (a e) -> p a e", a=4)),
                     reads=[bps], writes=[bLG])
        for a in range(16):
            S.op("dve", lambda h, a=a: h.max(out=M8[:, a, :], in_=LG[:, a, :]), reads=[bLG], writes=[bM8])
        S.op("dve", lambda h: h.tensor_scalar_mul(NM, M8[:, :, 0], -1.0), reads=[bM8], writes=[bM8])
        for a in range(16):
            S.op("act", lambda h, a=a: h.activation(out=EX[:, a, :], in_=LG[:, a, :], func=AF.Exp, bias=NM[:, a:a + 1], scale=1.0),
                 reads=[bLG, bM8], writes=[bEX])
        for a in range(16):
            S.op("dve", lambda h, a=a: h.tensor_single_scalar(LG[:, a, :], LG[:, a, :], M8[:, a, 1:2], op=ALU.is_ge),
                 reads=[bM8, bEX], writes=[bLG])
        S.op("dve", lambda h: h.tensor_mul(EX, EX, LG), reads=[bLG], writes=[bEX])
        S.op("dve", lambda h: h.reduce_sum(DEN, EX, axis=mybir.AxisListType.X), reads=[bEX], writes=[bM8])
        S.op("dve", lambda h: h.reciprocal(DEN, DEN), reads=[bM8], writes=[bM8])
        for a in range(16):
            S.op("dve", lambda h, a=a: h.tensor_scalar_mul(EX[:, a, :], EX[:, a, :], DEN[:, a:a + 1]),
                 reads=[bM8], writes=[bEX])
        for t in range(4):
            ps, bps = ps_next()
            for a4 in range(4):
                a = t * 4 + a4
                S.op("pe", lambda h, ps=ps, a=a, a4=a4: h.transpose(ps[0:8, a4 * 128:(a4 + 1) * 128], EX[:, a, :], IDN),
                     reads=[bEX, bCM], writes=[bps])
            S.op("act", lambda h, ps=ps, t=t: h.copy(GTt[:, t * TL:(t + 1) * TL], ps[0:8, :]), reads=[bps], writes=[bGTt])
        pend = {"fn": None, "n": 0, "m": 0}
        for e in range(NE):
            G, bG = Gb[e % 2], bGb[e % 2]
            for t in range(4):
                ps, bps = ps_next()
                S.op("pe", lambda h, ps=ps, e=e, t=t: h.matmul(ps, SEL[:, e * 128:(e + 1) * 128], GTt[:, t * TL:(t + 1) * TL],
                                                              start=True, stop=True), reads=[bSEL, bGTt], writes=[bps])
                S.op("act", lambda h, ps=ps, G=G, t=t: h.copy(G[:, t * TL:(t + 1) * TL], ps), reads=[bps], writes=[bG])
            swiglu(s_, w_mg, w_mu, w_md, e * D, e * FF, G, bG, AP_, bAP_, SGt, bSGt, UGt, bUGt, pend)
        pend["fn"]()
        S.barrier()
        A.release(m)

    def qkv_phase(blk_kv, blk_own, s0):
        s_ = 2
        norm_mod(s_, 4)
        m = A.mark()
        COS = A.alloc([128, TB], F32)
        SIN = A.alloc([128, TB], F32)
        bROPE = Buf()
        m1 = A.mark()
        HB = TB // 2
        PI_ = A.alloc([128, HB], I32)
        U = A.alloc([128, HB], F32)
        KI = A.alloc([128, HB], I32)
        KF = A.alloc([128, HB], F32)
        W2 = A.alloc([128, HB], F32)
        bT = gbuf("ropeT")
        for hb in range(2):
            c0 = hb * HB
            S.dma("sp", lambda h, c0=c0: h.dma_start(out=PI_, in_=poss[:, s0 + c0:s0 + c0 + HB].partition_broadcast(128)), writes=[bT])
            S.op("dve", lambda h: h.tensor_copy(U, PI_), reads=[bT], writes=[bT])
            S.op("dve", lambda h: h.tensor_scalar_mul(U, U, COLS[:, C_IF:C_IF + 1]), reads=[bT, bCOLS], writes=[bT])
            S.op("dve", lambda h: h.tensor_scalar_mul(U, U, 1.0 / TWO_PI), reads=[bT], writes=[bT])

            def frac_to(dst_scale_col, OUT, shift):
                S.op("dve", lambda h: h.tensor_scalar_add(W2, U, float(shift)), reads=[bT], writes=[bT])
                S.op("dve", lambda h: h.tensor_copy(KI, W2), reads=[bT], writes=[bT])
                S.op("dve", lambda h: h.tensor_copy(KF, KI), reads=[bT], writes=[bT])
                S.op("dve", lambda h: h.tensor_sub(W2, W2, KF), reads=[bT], writes=[bT])
                S.op("dve", lambda h: h.tensor_single_scalar(KF, W2, 0.5, op=ALU.is_gt), reads=[bT], writes=[bT])
                S.op("dve", lambda h: h.tensor_sub(W2, W2, KF), reads=[bT], writes=[bT])
                S.op("dve", lambda h: h.tensor_single_scalar(KF, W2, -0.5, op=ALU.is_lt), reads=[bT], writes=[bT])
                S.op("dve", lambda h: h.tensor_add(W2, W2, KF), reads=[bT], writes=[bT])
                S.op("act", lambda h: h.activation(out=OUT, in_=W2, func=AF.Sin, scale=TWO_PI), reads=[bT], writes=[bROPE])
                if dst_scale_col is not None:
                    S.op("dve", lambda h: h.tensor_scalar_mul(OUT, OUT, dst_scale_col), reads=[bROPE, bCOLS], writes=[bROPE])
            frac_to(COLS[:, C_SG:C_SG + 1], SIN[:, c0:c0 + HB], 0.0)
            frac_to(None, COS[:, c0:c0 + HB], 0.25)
        S.barrier()
        A.release(m1)
        T1 = [A.alloc([128, TL], F32) for _ in range(2)]
        bT1 = [Buf(), Buf()]
        T2 = [A.alloc([128, TL], F32) for _ in range(2)]
        bT2 = [Buf(), Buf()]
        OB = [A.alloc([128, TL], BF16) for _ in range(4)]
        bOB = [gbuf("ob%d" % i) for i in range(4)]
        cnt = 0
        ocnt = 0
        for c in range(KC):
            slot, bslot = ring_next()
            WQ = slot[:, 0:4 * KC * 128].rearrange("p (w k n) -> p w k n", w=4, k=KC)
            srcs = [w_q, w_qs, w_k, w_ks]
            wlist = [0, 1, 2, 3] if blk_own is not None else [2, 3]
            for w in wlist:
                wload(WQ[:, w], kview(srcs[w], c * 128, (c + 1) * 128), bslot)
            for t in range(4):
                for qk in ((0, 1) if blk_own is not None else (1,)):
                    psa, bpsa = ps_next()
                    psb, bpsb = ps_next()
                    for (ps, w) in ((psa, qk * 2), (psb, qk * 2 + 1)):
                        for k in range(KC):
                            S.op("pe", lambda h, ps=ps, w=w, k=k: h.matmul(
                                ps, WQ[:, w, k, :], hT[:, k, t * TL:(t + 1) * TL], start=(k == 0), stop=(k == KC - 1)),
                                reads=[bslot, bhT[k][t]], writes=[bpsa if ps is psa else bpsb])
                    t1, bt1 = T1[cnt % 2], bT1[cnt % 2]
                    t2, bt2 = T2[cnt % 2], bT2[cnt % 2]
                    cnt += 1
                    ob, bob = OB[ocnt % 4], bOB[ocnt % 4]
                    ocnt += 1
                    S.op("dve", lambda h, t1=t1, psa=psa: h.tensor_tensor(t1, psa, COS[:, t * TL:(t + 1) * TL], op=ALU.mult),
                         reads=[bpsa, bROPE], writes=[bt1])
                    S.op("dve", lambda h, t2=t2, psb=psb: h.tensor_tensor(t2, psb, SIN[:, t * TL:(t + 1) * TL], op=ALU.mult),
                         reads=[bpsb, bROPE], writes=[bt2])
                    S.op("dve", lambda h, ob=ob, t1=t1, t2=t2: h.tensor_tensor(ob, t1, t2, op=ALU.add),
                         reads=[bt1, bt2], writes=[bob])
                    if qk == 0:
                        col0 = blk_own * TB + t * TL
                        dst = QT[c * 128:(c + 1) * 128, col0:col0 + TL]
                        if DBG.get("noqstore"):
                            continue
                        S.dma("sp", lambda h, dst=dst, ob=ob: h.dma_start(out=dst, in_=ob), reads=[bob], writes=[bQT[c][blk_own]])
                    else:
                        col0 = blk_kv * TB + t * TL
                        dst = KT[c * 128:(c + 1) * 128, col0:col0 + TL]
                        S.dma("sp", lambda h, dst=dst, ob=ob: h.dma_start(out=dst, in_=ob), reads=[bob], writes=[bKT[c][blk_kv]])
        for half in range(2):
            slot, bslot = ring_next()
            WV = slot[:, 0:KC * 512].rearrange("p (k n) -> p k n", k=KC)
            wload(WV, kview(w_v, half * 512, (half + 1) * 512), bslot)
            for a in range(16):
                t = a // 4
                ps, bps = ps_next()
                for k in range(KC):
                    S.op("pe", lambda h, ps=ps, k=k, a=a: h.matmul(
                        ps, hT[:, k, a * 128:(a + 1) * 128], WV[:, k, :], start=(k == 0), stop=(k == KC - 1)),
                        reads=[bslot, bhT[k][t]], writes=[bps])
                ob, bob = OB[ocnt % 4], bOB[ocnt % 4]
                ocnt += 1
                S.op("act", lambda h, ob=ob, ps=ps: h.copy(ob, ps), reads=[bps], writes=[bob])
                r0 = blk_kv * TB + a * 128
                dst = VV[r0:r0 + 128, half * 512:(half + 1) * 512]
                S.dma("sp", lambda h, dst=dst, ob=ob: h.dma_start(out=dst, in_=ob), reads=[bob], writes=[bVV[blk_kv]])
        S.barrier()
        A.release(m)

    def attn_phase(blk_own):
        blk_kv = blk_own + 1
        s_ = 2
        m = A.mark()
        QH = [A.alloc([64, TB], BF16) for _ in range(2)]
        KH = [A.alloc([64, 2 * TB], BF16) for _ in range(2)]
        VH = [A.alloc([128, 32, 64], BF16) for _ in range(2)]
        bHD = [gbuf("hd0"), gbuf("hd1")]
        KD = A.alloc([64, 2 * TB], BF16); bKD = Buf()
        A2 = Arena(nc, hT_addr, hT_addr + KC * TB * 2)
        A2.n = 9000 + blk_own * 10
        OTs = A2.alloc([64, 3, TB], F32); bOTs = Buf()
        RSUM = A2.alloc([64, TB], F32); bRSUM = Buf()
        ATs = [A.alloc([64, TB], BF16) for _ in range(2)]
        bATs = [gbuf("ats0"), gbuf("ats1")]
        PT = [A.alloc([128, 256], BF16) for _ in range(3)]
        bPT = [Buf() for _ in range(3)]
        PM = [A.alloc([128, 256], BF16) for _ in range(3)]
        bPM = [Buf() for _ in range(3)]
        dil = (1, 4, 16)
        hcnt = 0
        pcnt = 0
        acnt = 0

        def load_head(hd, g, i):
            d = dil[g]
            r0 = hd * 64
            S.dma("sp", lambda h: h.dma_start(out=QH[i], in_=QT[r0:r0 + 64, blk_own * TB:(blk_own + 1) * TB]),
                  reads=[bQT[hd // 2][blk_own]], writes=[bHD[i]])
            S.dma("sp", lambda h: h.dma_start(out=KH[i], in_=KT[r0:r0 + 64, (blk_kv - 1) * TB:(blk_kv + 1) * TB]),
                  reads=[bKT[hd // 2][blk_kv - 1], bKT[hd // 2][blk_kv]], writes=[bHD[i]])
            nchunk = 2 * TB // (128 * d)
            vsrc = VV[(blk_kv - 1) * TB:(blk_kv + 1) * TB, r0:r0 + 64]
            if d == 1:
                src = vsrc.rearrange("(n i) c -> i n c", i=128)
                for q in range(4):
                    S.dma("sp", lambda h, q=q: h.dma_start(out=VH[i][:, q * 8:(q + 1) * 8, :], in_=src[:, q * 8:(q + 1) * 8, :]),
                          reads=[bVV[blk_kv - 1], bVV[blk_kv]], writes=[bHD[i]])
            else:
                src = vsrc.rearrange("(n i r) c -> n i r c", i=128, r=d)
                for n_ in range(nchunk):
                    S.dma("sp", lambda h, n_=n_: h.dma_start(out=VH[i][:, n_ * d:(n_ + 1) * d, :], in_=src[n_]),
                          reads=[bVV[blk_kv - 1], bVV[blk_kv]], writes=[bHD[i]])

        order = [(hh, g) for hh in range(5) for g in range(3)]
        load_head(order[0][1] * 5 + order[0][0], order[0][1], 0)
        for oi, (hh, g) in enumerate(order):
            if stop_after is not None and stop_after == "A_heads%d" % oi and blk_own == 0:
                S.barrier()
                stage(stop_after)
            hd = g * 5 + hh
            d = dil[g]
            i = oi % 2
            if oi + 1 < len(order):
                hh2, g2 = order[oi + 1]
                load_head(g2 * 5 + hh2, g2, (oi + 1) % 2)
            qh, kh, vh, bhd = QH[i], KH[i], VH[i], bHD[i]
            if d > 1:
                S.op("act", lambda h, kh=kh, d=d: h.copy(KD.rearrange("p (r i) -> p r i", r=d), kh.rearrange("p (i r) -> p r i", r=d)),
                     reads=[bhd], writes=[bKD])
                kst, bkst = KD, bKD
            else:
                kst, bkst = kh, bhd
            npair = 0 if DBG.get("nocomp") else 16
            for pr in range(npair):
                n, r = pr // d, pr % d
                sl = lambda st0: slice(st0, st0 + 127 * d + 1, d)
                qsl = sl(n * 128 * d + r)
                ncls = 2 * TB // d
                i0 = TB // d + 128 * n
                if d > 1:
                    kcur = slice(r * ncls + i0, r * ncls + i0 + 128)
                    kprv = slice(r * ncls + i0 - 128, r * ncls + i0)
                else:
                    kcur = slice(TB + n * 128, TB + n * 128 + 128)
                    kprv = slice(TB + (n - 1) * 128, TB + n * 128)
                set_cur = 16 + n * d + r
                set_prv = set_cur - d
                pss, bpss = ps_next()
                S.op("pe", lambda h, pss=pss, kst=kst, qh=qh, kprv=kprv, qsl=qsl: h.matmul(
                    pss[:, 0:128], kst[:, kprv], qh[:, qsl], start=True, stop=True), reads=[bhd, bkst], writes=[bpss])
                S.op("pe", lambda h, pss=pss, kst=kst, qh=qh, kcur=kcur, qsl=qsl: h.matmul(
                    pss[:, 128:256], kst[:, kcur], qh[:, qsl], start=True, stop=True), reads=[bhd, bkst], writes=[bpss])
                pt, bpt = PT[pcnt % 3], bPT[pcnt % 3]
                pm, bpm = PM[pcnt % 3], bPM[pcnt % 3]
                pcnt += 1
                S.op("act", lambda h, pt=pt, pss=pss: h.activation(out=pt, in_=pss[:, 0:256], func=AF.Exp, scale=0.125),
                     reads=[bpss], writes=[bpt])
                mk = MASKH if (blk_own == 0 and n == 0) else MASK
                S.op("dve", lambda h, pm=pm, pt=pt, mk=mk: h.tensor_tensor(pm, pt, mk, op=ALU.mult),
                     reads=[bpt, bMASK], writes=[bpm])
                pso, bpso = ps_next()
                S.op("pe", lambda h, pso=pso, vh=vh, pm=pm, set_prv=set_prv: h.matmul(
                    pso[0:64, 0:128], vh[:, set_prv, :], pm[:, 0:128], start=True, stop=False), reads=[bhd, bpm], writes=[bpso])
                S.op("pe", lambda h, pso=pso, vh=vh, pm=pm, set_cur=set_cur: h.matmul(
                    pso[0:64, 0:128], vh[:, set_cur, :], pm[:, 128:256], start=False, stop=True), reads=[bhd, bpm], writes=[bpso])
                S.op("pe", lambda h, pso=pso, pm=pm: h.matmul(
                    pso[0:64, 128:256], ONESB, pm[:, 0:128], start=True, stop=False), reads=[bONES, bpm], writes=[bpso])
                S.op("pe", lambda h, pso=pso, pm=pm: h.matmul(
                    pso[0:64, 128:256], ONESB, pm[:, 128:256], start=False, stop=True), reads=[bONES, bpm], writes=[bpso])
                S.op("act", lambda h, pso=pso, g=g, qsl=qsl: h.copy(OTs[:, g, qsl], pso[0:64, 0:128]), reads=[bpso], writes=[bOTs])
                if g == 0:
                    S.op("dve", lambda h, pso=pso, qsl=qsl: h.tensor_copy(RSUM[:, qsl], pso[0:64, 128:256]),
                         reads=[bpso], writes=[bRSUM])
                else:
                    S.op("dve", lambda h, pso=pso, qsl=qsl: h.tensor_tensor(RSUM[:, qsl], pso[0:64, 128:256], RSUM[:, qsl], op=ALU.add),
                         reads=[bpso], writes=[bRSUM])
            if g == 2:
                S.op("dve", lambda h: h.reciprocal(RSUM, RSUM), reads=[bRSUM], writes=[bRSUM])
                for g3 in range(3):
                    ats, bats = ATs[acnt % 2], bATs[acnt % 2]
                    acnt += 1
                    S.op("dve", lambda h, ats=ats, g3=g3: h.tensor_tensor(ats, OTs[:, g3, :], RSUM, op=ALU.mult),
                         reads=[bOTs, bRSUM], writes=[bats])
                    hd3 = g3 * 5 + hh
                    dst = AT[hd3 * 64:(hd3 + 1) * 64, blk_own * TB:(blk_own + 1) * TB]
                    S.dma("sp", lambda h, dst=dst, ats=ats: h.dma_start(out=dst, in_=ats), reads=[bats], writes=[bAT[hd3][blk_own]])
        S.barrier()
        A.release(m)
        for c in range(KC):
            rows = 128 if c < 7 else 64
            src = AT[c * 128:c * 128 + rows, blk_own * TB:(blk_own + 1) * TB]
            deps = [bAT[2 * c][blk_own]] + ([bAT[2 * c + 1][blk_own]] if c < 7 else [])
            S.dma("sp", lambda h, c=c, rows=rows, src=src: h.dma_start(out=hT[0:rows, c, :], in_=src),
                  reads=deps, writes=[gbuf("atl")] + [bhT[c2][t2] for c2 in range(c + 1) for t2 in range(4)])
        for dg in range(2):
            slot, bslot = ring_next()
            WO_ = slot[:, 0:KC * 512].rearrange("p (k n) -> p k n", k=KC)
            wload(WO_, kview(w_o, dg * 512, (dg + 1) * 512), bslot)
            for t in range(4):
                for dl in range(4):
                    dch = dg * 4 + dl
                    ps, bps = ps_next()
                    for c in range(KC):
                        rows = 128 if c < 7 else 64
                        S.op("pe", lambda h, ps=ps, c=c, dl=dl, rows=rows: h.matmul(
                            ps, WO_[0:rows, c, dl * 128:(dl + 1) * 128], hT[0:rows, c, t * TL:(t + 1) * TL],
                            start=(c == 0), stop=(c == KC - 1)), reads=[bslot, bhT[c][t]], writes=[bps])
                    resid_add(ps, bps, s_, dch, t)
        S.barrier()

    def final_phase(blk_own):
        m = A.mark()
        SQ = [A.alloc([128, TL], F32) for _ in range(2)]
        bSQ = [Buf(), Buf()]
        RS = [A.alloc([128, TL], F32) for _ in range(2)]
        bRS = [Buf(), Buf()]
        YT = [A.alloc([128, KC, TL], F32) for _ in range(2)]
        bYT = [Buf(), Buf()]
        OS = [A.alloc([128, D], F32) for _ in range(3)]
        bOS = [gbuf("os%d" % i) for i in range(3)]
        oc = 0
        outs = []
        for t in range(4):
            rs, brs = RS[t % 2], bRS[t % 2]
            yt, byt = YT[t % 2], bYT[t % 2]
            norm_stats(t, SQ, bSQ, rs, brs)
            for k in range(KC):
                S.op("dve", lambda h, yt=yt, k=k, rs=rs: h.scalar_tensor_tensor(
                    out=yt[:, k, :], in0=xT[:, k, t * TL:(t + 1) * TL], scalar=COLS[:, C_FG + k:C_FG + k + 1], in1=rs,
                    op0=ALU.mult, op1=ALU.mult), reads=[bxT[k][t], brs, bCOLS], writes=[byt])
            for a in range(4):
                os_, bos = OS[oc % 3], bOS[oc % 3]
                oc += 1
                for half in range(2):
                    ps, bps = ps_next()
                    for kk in range(4):
                        k = half * 4 + kk
                        S.op("pe", lambda h, ps=ps, yt=yt, k=k, kk=kk, a=a: h.transpose(
                            ps[:, kk * 128:(kk + 1) * 128], yt[:, k, a * 128:(a + 1) * 128], IDN),
                            reads=[byt, bCM], writes=[bps])
                    if half == 0:
                        S.op("act", lambda h, ps=ps, os_=os_: h.copy(os_[:, 0:512], ps), reads=[bps], writes=[bos])
                    else:
                        S.op("dve", lambda h, ps=ps, os_=os_: h.tensor_copy(os_[:, 512:1024], ps), reads=[bps], writes=[bos])
                r0 = blk_own * TB + t * TL + a * 128
                S.dma("sp", lambda h, os_=os_, r0=r0: h.dma_start(out=out[r0:r0 + 128, :], in_=os_), reads=[bos])
                outs.append(bos)
        S.final_wait("sp", outs)
        S.barrier()
        A.release(m)

    class Stop(Exception):
        pass

    def stage(name):
        if stop_after == name:
            dbg = nc.dram_tensor("dbg", [D, TB], F32, kind="ExternalOutput").ap()
            bd = Buf()
            S.dma("sp", lambda h: h.dma_start(out=dbg.rearrange("(k p) t -> p k t", p=128), in_=xT),
                  reads=[b_ for row in bxT for b_ in row], writes=[bd])
            S.final_wait("sp", [bd])
            raise Stop()

    try:
        load_x(0, 1)
        norm_mod(0, 1)
        conv_phase(1, True, False)
        for blk in range(3):
            bn = "HAB"[blk]
            s0 = 512 + blk * TB
            load_x(s0, 4)
            stage(bn + "_load")
            m_c = A.mark()
            norm_mod(0, 4, keep=True)
            if stop_after == bn + "_norm":
                for k in range(KC):
                    for t in range(4):
                        S.op("dve", lambda h, k=k, t=t: h.tensor_copy(xT[:, k, t * TL:(t + 1) * TL], hT[:, k, t * TL:(t + 1) * TL]),
                             reads=[bhT[k][t]], writes=[bxT[k][t]])
                S.op("dve", lambda h: h.tensor_copy(xT[:, 0, 0:96], MODC), reads=[bMODC], writes=[bxT[0][0]])
                S.op("dve", lambda h: h.tensor_copy(xT[:, 0, 96:128], GS), reads=[bMODC], writes=[bxT[0][0]])
                stage(bn + "_norm")
            conv_phase(4, False, blk == 1)
            A.release(m_c)
            stage(bn + "_conv")
            ffn_phase()
            stage(bn + "_ffn")
            qkv_phase(blk, None if blk == 0 else blk - 1, s0)
            stage(bn + "_kv")
            if blk == 0:
                continue
            attn_phase(blk - 1)
            stage(bn + "_attn")
            moe_phase()
            stage(bn + "_moe")
            final_phase(blk - 1)
    except Stop:
        pass
    stats = S.emit(st)
    return nc, stats, st, used


_CACHE = {}
DBG = {}


def _prep_inputs(x, c, positions, mod_w, mod_b, norm_g, conv_w_in, conv_w, conv_w_out,
                 ffn_w_gate, ffn_w_up, ffn_w_down, attn_w_qkv, attn_w_o, router_w,
                 moe_w_gate, moe_w_up, moe_w_down, final_g):
    f32 = np.float32
    x = np.asarray(x, f32); c = np.asarray(c, f32); positions = np.asarray(positions, np.int32)

    def colmajor(v):
        v = np.asarray(v, f32)
        return np.ascontiguousarray(v.reshape(-1, 128).T)

    wqkv = np.asarray(attn_w_qkv, f32)[0]
    AW = 960

    def pad_cols(w):
        o = np.zeros((D, D), f32)
        o[:, :AW] = w
        return o

    def swap_halves(w):
        w4 = w.reshape(D, 15, 2, 32)
        return np.ascontiguousarray(w4[:, :, ::-1, :]).reshape(D, AW)
    wq, wk, wv = wqkv[:, 0:AW], wqkv[:, AW:2 * AW], wqkv[:, 2 * AW:3 * AW]
    wo = np.zeros((D, D), f32)
    wo[:AW, :] = np.asarray(attn_w_o, f32)[0]
    shared = {
        "modw": np.ascontiguousarray(np.asarray(mod_w, f32).reshape(4 * D, 3 * D)),
        "w_cin": np.ascontiguousarray(np.asarray(conv_w_in, f32)[0]),
        "w_cout": np.ascontiguousarray(np.asarray(conv_w_out, f32)[0]),
        "w_fg": np.ascontiguousarray(np.asarray(ffn_w_gate, f32)[0]),
        "w_fu": np.ascontiguousarray(np.asarray(ffn_w_up, f32)[0]),
        "w_fd": np.ascontiguousarray(np.asarray(ffn_w_down, f32)[0]),
        "w_q": pad_cols(wq), "w_qs": pad_cols(swap_halves(wq)),
        "w_k": pad_cols(wk), "w_ks": pad_cols(swap_halves(wk)),
        "w_v": pad_cols(wv), "w_o": wo,
        "w_r": np.ascontiguousarray(np.asarray(router_w, f32)[0]),
        "w_mg": np.ascontiguousarray(np.asarray(moe_w_gate, f32).reshape(NE * D, FF)),
        "w_mu": np.ascontiguousarray(np.asarray(moe_w_up, f32).reshape(NE * D, FF)),
        "w_md": np.ascontiguousarray(np.asarray(moe_w_down, f32).reshape(NE * FF, D)),
    }
    cm = np.zeros((128, 448), f32)
    cm[:, 0:128] = np.eye(128, dtype=f32)
    ik = np.arange(128)[:, None]
    aq = np.arange(128)[None, :]
    cm[:, 128:256] = (ik >= aq)
    cm[:, 256:384] = (ik <= aq)
    selm = np.zeros((8, NE * 128), f32)
    for e in range(NE):
        selm[e, e * 128:(e + 1) * 128] = 1.0
    shared["cmat"] = cm
    shared["sel"] = selm
    p = np.arange(128)
    inv_freq = (10000.0 ** (-(np.arange(0, 64, 2, dtype=np.float32)) / np.float32(64))).astype(f32)
    in_maps = []
    ng = np.asarray(norm_g, f32).reshape(4, D)
    mb = np.asarray(mod_b, f32).reshape(4, 3 * D)
    cw = np.asarray(conv_w, f32)[0]
    for core in range(8):
        b, half = core // 2, core % 2
        base = half * NOWN
        lo = base - 2560
        xsb = np.zeros((NS, D), f32)
        ps_ = np.zeros((1, NS), np.int32)
        v0 = max(lo, 0)
        xsb[v0 - lo:] = x[b, v0:base + NOWN]
        ps_[0, v0 - lo:] = positions[b, v0:base + NOWN]
        colsb = np.zeros((128, 176), f32)
        for s_ in range(4):
            colsb[:, s_ * 8:(s_ + 1) * 8] = colmajor(ng[s_])
        colsb[:, 32:40] = colmajor(final_g)
        for tap in range(3):
            colsb[:, 40 + tap * 8:48 + tap * 8] = colmajor(cw[tap])
        colsb[:, 64] = inv_freq[p % 32]
        colsb[:, 65] = np.where((p % 64) < 32, -1.0, 1.0)
        colsb[:, 66] = float(half)
        colsb[:, 67:75] = colmajor(c[b])
        for s_ in range(4):
            colsb[:, 75 + s_ * 24:75 + (s_ + 1) * 24] = colmajor(mb[s_])
        mp = dict(shared)
        mp["xs"] = xsb
        mp["poss"] = ps_
        mp["cols"] = colsb
        in_maps.append(mp)
    return in_maps


def kernel(**inputs):
    if "nc" not in _CACHE:
        nc, stats, st, used = build_program()
        _CACHE["nc"] = nc
        _CACHE["st"] = st
        _CACHE["used"] = used
    nc = _CACHE["nc"]
    in_maps = _prep_inputs(**inputs)
    in_maps = [{k: v for k, v in m.items() if k in _CACHE["used"]} for m in in_maps]
    res = run_bass_kernel_spmd(nc, in_maps, core_ids=list(range(8)))
    outp = np.zeros((4, 8192, D), np.float32)
    for core in range(8):
        b, half = core // 2, core % 2
        outp[b, half * NOWN:(half + 1) * NOWN] = res.results[core]["out"]
    return outp
```
